# Optimizing a Trainium2 kernel written in Bass

```python
import math
import jax, jax.numpy as jnp
from jax import lax
import numpy as np

D_MODEL = 1024
BATCH = 16
SEQ = 2048
DEPTH = 4

CHUNK = 64
N_MEM = 256
MIX_WIDTH = D_MODEL
M_WIDTH = MIX_WIDTH // 2
M_HEADS = 4
M_HDIM = M_WIDTH // M_HEADS
M_CONV = 4
F_BIAS_LO = 3.0
F_BIAS_HI = 6.0
R_WIDTH = MIX_WIDTH - M_WIDTH
R_HDIM = 64
R_HEADS = R_WIDTH // R_HDIM
R_DECAY_LORA = 64
R_AAA_LORA = 64
R_GATE_LORA = 160
DECAY_SCALE = math.exp(-0.5)
X_HEADS = 4
X_HDIM = D_MODEL // X_HEADS
D_FF = 2816
FFN_CONV = 3
NORM_EPS = 1e-6
GN_EPS = 64e-5
M_COLS = 4 * M_WIDTH + 2 * M_HEADS
R_COLS = 3 * R_WIDTH + R_DECAY_LORA + R_AAA_LORA + R_GATE_LORA
IN_COLS = M_COLS + R_COLS

kernel_name = 'hybrid_mlstm_rwkv7_encoder'


def rmsnorm(x, g, eps=NORM_EPS):
    xf = x.astype(jnp.float32)
    y = xf * lax.rsqrt(jnp.mean(xf * xf, -1, keepdims=True) + eps)
    return (y * g.astype(jnp.float32)).astype(x.dtype)


def standardize_heads(y, eps):
    yf = y.astype(jnp.float32)
    mu = jnp.mean(yf, -1, keepdims=True)
    var = jnp.mean(jnp.square(yf - mu), -1, keepdims=True)
    return (yf - mu) * lax.rsqrt(var + eps)


def causal_dwconv(x, w, b):
    width = w.shape[0]
    seq = x.shape[1]
    xp = jnp.pad(x, ((0, 0), (width - 1, 0), (0, 0)))
    y = b
    for j in range(width):
        y = y + xp[:, j:j + seq] * w[j]
    return y


def token_shift(p, mu):
    prev = jnp.pad(p, ((0, 0), (1, 0), (0, 0)))[:, :-1]
    return p + (prev - p) * mu


def mlstm_chunkwise(q, k, v, i_pre, f_pre):
    B, H, S, dh = q.shape
    nc = S // CHUNK
    f32 = jnp.float32
    q = q.astype(f32).reshape(B, H, nc, CHUNK, dh) * (dh ** -0.5)
    k = k.astype(f32).reshape(B, H, nc, CHUNK, dh)
    v = v.astype(f32).reshape(B, H, nc, CHUNK, dh)
    logi = i_pre.astype(f32).reshape(B, H, nc, CHUNK)
    logf = jax.nn.log_sigmoid(f_pre.astype(f32)).reshape(B, H, nc, CHUNK)
    b = jnp.cumsum(logf, -1)
    g = b[..., -1]
    a = g[..., None] - b + logi
    m_loc = jnp.max(a, -1)
    wa = jnp.exp(a - m_loc[..., None])
    c_loc = jnp.einsum('bhclv,bhclk->bhcvk', v * wa[..., None], k)
    n_loc = jnp.einsum('bhcl,bhclk->bhck', wa, k)

    def step(carry, inp):
        c, n, m = carry
        g_c, m_l, c_l, n_l = inp
        m_new = jnp.maximum(g_c + m, m_l)
        s_old = jnp.exp(g_c + m - m_new)
        s_loc = jnp.exp(m_l - m_new)
        c_new = s_old[..., None, None] * c + s_loc[..., None, None] * c_l
        n_new = s_old[..., None] * n + s_loc[..., None] * n_l
        return (c_new, n_new, m_new), (c, n, m)

    init = (jnp.zeros((B, H, dh, dh), f32), jnp.zeros((B, H, dh), f32), jnp.zeros((B, H), f32))
    xs = (jnp.moveaxis(g, 2, 0), jnp.moveaxis(m_loc, 2, 0),
          jnp.moveaxis(c_loc, 2, 0), jnp.moveaxis(n_loc, 2, 0))
    _, (c_prev, n_prev, m_prev) = lax.scan(step, init, xs)
    c_prev = jnp.moveaxis(c_prev, 0, 2)
    n_prev = jnp.moveaxis(n_prev, 0, 2)
    m_prev = jnp.moveaxis(m_prev, 0, 2)

    inter = b + m_prev[..., None]
    causal = jnp.tril(jnp.ones((CHUNK, CHUNK), dtype=bool))
    d = jnp.where(causal, b[..., :, None] - b[..., None, :] + logi[..., None, :], -jnp.inf)
    m_t = jnp.maximum(inter, jnp.max(d, -1))
    s_int = jnp.exp(inter - m_t)
    p = jnp.exp(d - m_t[..., None]) * jnp.einsum('bhctd,bhcsd->bhcts', q, k)
    num = (s_int[..., None] * jnp.einsum('bhcvk,bhctk->bhctv', c_prev, q)
           + jnp.einsum('bhcts,bhcsv->bhctv', p, v))
    den = s_int * jnp.einsum('bhck,bhctk->bhct', n_prev, q) + jnp.sum(p, -1)
    h = num / jnp.maximum(jnp.abs(den), jnp.exp(-m_t))[..., None]
    return h.reshape(B, H, S, dh)


def rwkv7_scan(r, w, k, v, kk, a):
    B, S, H, N = r.shape

    def step(state, inp):
        r_t, w_t, k_t, v_t, kk_t, a_t = inp
        sk = jnp.einsum('bhvk,bhk->bhv', state, kk_t)
        state = (state * w_t[:, :, None, :]
                 - sk[..., None] * (kk_t * a_t)[:, :, None, :]
                 + v_t[..., None] * k_t[:, :, None, :])
        y = jnp.einsum('bhvk,bhk->bhv', state, r_t)
        return state, y

    xs = (jnp.moveaxis(r, 1, 0), jnp.moveaxis(w, 1, 0), jnp.moveaxis(k, 1, 0),
          jnp.moveaxis(v, 1, 0), jnp.moveaxis(kk, 1, 0), jnp.moveaxis(a, 1, 0))
    _, y = lax.scan(step, jnp.zeros((B, H, N, N), jnp.float32), xs)
    return jnp.moveaxis(y, 0, 1)


def hybrid_token_mixer(h, w_in, m_conv_w, m_conv_b, m_gate_b, m_norm_g, r_mu, r_w0, r_w_up,
                       r_a0, r_a_up, r_g_up, r_kk, r_ka, r_rk, r_gn_g, r_gn_b, w_out):
    B, S, _ = h.shape
    f32 = jnp.float32
    p = h @ w_in
    pm, pr = p[..., :M_COLS], p[..., M_COLS:]

    qk = jax.nn.silu(causal_dwconv(pm[..., :2 * M_WIDTH], m_conv_w, m_conv_b))
    q, k = qk[..., :M_WIDTH], qk[..., M_WIDTH:]
    v = pm[..., 2 * M_WIDTH:3 * M_WIDTH]
    o = pm[..., 3 * M_WIDTH:4 * M_WIDTH]
    gates = pm[..., 4 * M_WIDTH:] + m_gate_b
    i_pre, f_pre = gates[..., :M_HEADS], gates[..., M_HEADS:]

    def to_heads(t):
        return t.reshape(B, S, M_HEADS, M_HDIM).transpose(0, 2, 1, 3)

    hm = mlstm_chunkwise(to_heads(q), to_heads(k), to_heads(v),
                         i_pre.transpose(0, 2, 1), f_pre.transpose(0, 2, 1))
    hm = standardize_heads(hm.transpose(0, 2, 1, 3), NORM_EPS).reshape(B, S, M_WIDTH)
    y_m = (jax.nn.sigmoid(o.astype(f32)) * hm * m_norm_g.astype(f32)).astype(h.dtype)

    pr = token_shift(pr, r_mu)
    rr = pr[..., :R_WIDTH]
    kr = pr[..., R_WIDTH:2 * R_WIDTH]
    vr = pr[..., 2 * R_WIDTH:3 * R_WIDTH]
    off = 3 * R_WIDTH
    wd = pr[..., off:off + R_DECAY_LORA]
    off = off + R_DECAY_LORA
    ad = pr[..., off:off + R_AAA_LORA]
    off = off + R_AAA_LORA
    gd = pr[..., off:off + R_GATE_LORA]
    w = jnp.exp(-DECAY_SCALE * jax.nn.sigmoid((r_w0 + jnp.tanh(wd) @ r_w_up).astype(f32)))
    a = jax.nn.sigmoid((r_a0 + ad @ r_a_up).astype(f32))
    g = jax.nn.sigmoid(gd) @ r_g_up

    def rh(t):
        return t.astype(f32).reshape(B, S, R_HEADS, R_HDIM)

    kk = rh(kr * r_kk)
    kk = kk / jnp.maximum(jnp.sqrt(jnp.sum(kk * kk, -1, keepdims=True)), 1e-12)
    kr = kr.astype(f32) * (1.0 + (a - 1.0) * r_ka.astype(f32))
    rr, kr, vr, w, a = rh(rr), rh(kr), rh(vr), rh(w), rh(a)
    yr = rwkv7_scan(rr, w, kr, vr, kk, a)
    yr = (standardize_heads(yr, GN_EPS) * r_gn_g.astype(f32).reshape(R_HEADS, R_HDIM)
          + r_gn_b.astype(f32).reshape(R_HEADS, R_HDIM))
    yr = yr + jnp.sum(rr * kr * r_rk.astype(f32), -1, keepdims=True) * vr
    y_r = (yr.reshape(B, S, R_WIDTH) * g.astype(f32)).astype(h.dtype)

    return jnp.concatenate([y_m, y_r], -1) @ w_out


def mem_cross_attn(h, memn, w_q, w_kv, w_o):
    B, S, _ = h.shape
    M = memn.shape[1]
    q = (h @ w_q).reshape(B, S, X_HEADS, X_HDIM)
    kv = memn @ w_kv
    k = kv[..., :D_MODEL].reshape(B, M, X_HEADS, X_HDIM)
    v = kv[..., D_MODEL:].reshape(B, M, X_HEADS, X_HDIM)
    s = jnp.einsum('bshd,bmhd->bhsm', q, k).astype(jnp.float32) * (X_HDIM ** -0.5)
    pr = jax.nn.softmax(s, -1).astype(v.dtype)
    o = jnp.einsum('bhsm,bmhd->bshd', pr, v).reshape(B, S, D_MODEL)
    return o @ w_o


def conv_glu_ffn(h, w_up, conv_w, conv_b, w_down):
    u = h @ w_up
    gate, val = u[..., :D_FF], u[..., D_FF:]
    gate = causal_dwconv(gate, conv_w, conv_b)
    return (jax.nn.silu(gate) * val) @ w_down


def setup_inputs(seed: int = 0) -> dict:
    key = jax.random.key(seed)
    ks = iter(jax.random.split(key, 48))
    L = DEPTH

    def nrm(shape, scale):
        return jax.random.normal(next(ks), shape, jnp.float32) * scale

    def gain(shape):
        return 1.0 + nrm(shape, 0.02)

    def unif(shape, lo, hi):
        return jax.random.uniform(next(ks), shape, jnp.float32, minval=lo, maxval=hi)

    x = nrm((BATCH, SEQ, D_MODEL), 1.0)
    mem = nrm((BATCH, N_MEM, D_MODEL), 1.0)
    f_bias = jnp.linspace(F_BIAS_LO, F_BIAS_HI, M_HEADS, dtype=jnp.float32)
    m_gate_b = jnp.concatenate([nrm((L, M_HEADS), 0.1), f_bias + nrm((L, M_HEADS), 0.1)], -1)
    return {
        'x': x,
        'mem': mem,
        'norm_mix': gain((L, D_MODEL)),
        'w_in': nrm((L, D_MODEL, IN_COLS), D_MODEL ** -0.5),
        'm_conv_w': nrm((L, M_CONV, 2 * M_WIDTH), M_CONV ** -0.5),
        'm_conv_b': nrm((L, 2 * M_WIDTH), 0.02),
        'm_gate_b': m_gate_b,
        'm_norm_g': gain((L, M_WIDTH)),
        'r_mu': unif((L, R_COLS), 0.0, 1.0),
        'r_w0': unif((L, R_WIDTH), -2.0, 2.0),
        'r_w_up': nrm((L, R_DECAY_LORA, R_WIDTH), R_DECAY_LORA ** -0.5),
        'r_a0': nrm((L, R_WIDTH), 0.1),
        'r_a_up': nrm((L, R_AAA_LORA, R_WIDTH), R_AAA_LORA ** -0.5),
        'r_g_up': nrm((L, R_GATE_LORA, R_WIDTH), R_GATE_LORA ** -0.5),
        'r_kk': 0.85 + nrm((L, R_WIDTH), 0.05),
        'r_ka': 1.0 + nrm((L, R_WIDTH), 0.05),
        'r_rk': nrm((L, R_HEADS, R_HDIM), 0.1),
        'r_gn_g': gain((L, R_WIDTH)),
        'r_gn_b': nrm((L, R_WIDTH), 0.02),
        'w_out': nrm((L, MIX_WIDTH, D_MODEL), MIX_WIDTH ** -0.5),
        'norm_x': gain((L, D_MODEL)),
        'norm_mem': gain((L, D_MODEL)),
        'x_wq': nrm((L, D_MODEL, D_MODEL), D_MODEL ** -0.5),
        'x_wkv': nrm((L, D_MODEL, 2 * D_MODEL), D_MODEL ** -0.5),
        'x_wo': nrm((L, D_MODEL, D_MODEL), D_MODEL ** -0.5),
        'norm_ffn': gain((L, D_MODEL)),
        'f_up': nrm((L, D_MODEL, 2 * D_FF), D_MODEL ** -0.5),
        'f_conv_w': nrm((L, FFN_CONV, D_FF), FFN_CONV ** -0.5),
        'f_conv_b': nrm((L, D_FF), 0.02),
        'f_down': nrm((L, D_FF, D_MODEL), D_FF ** -0.5),
        'norm_final': gain((D_MODEL,)),
    }


def reference(x, mem, norm_mix, w_in, m_conv_w, m_conv_b, m_gate_b, m_norm_g, r_mu, r_w0,
              r_w_up, r_a0, r_a_up, r_g_up, r_kk, r_ka, r_rk, r_gn_g, r_gn_b, w_out,
              norm_x, norm_mem, x_wq, x_wkv, x_wo, norm_ffn, f_up, f_conv_w, f_conv_b,
              f_down, norm_final):
    for l in range(DEPTH):
        h = rmsnorm(x, norm_mix[l])
        x = x + hybrid_token_mixer(h, w_in[l], m_conv_w[l], m_conv_b[l], m_gate_b[l],
                                   m_norm_g[l], r_mu[l], r_w0[l], r_w_up[l], r_a0[l],
                                   r_a_up[l], r_g_up[l], r_kk[l], r_ka[l], r_rk[l],
                                   r_gn_g[l], r_gn_b[l], w_out[l])
        h = rmsnorm(x, norm_x[l])
        memn = rmsnorm(mem, norm_mem[l])
        x = x + mem_cross_attn(h, memn, x_wq[l], x_wkv[l], x_wo[l])
        h = rmsnorm(x, norm_ffn[l])
        x = x + conv_glu_ffn(h, f_up[l], f_conv_w[l], f_conv_b[l], f_down[l])
    return rmsnorm(x, norm_final)
```

```python
import math
from contextlib import ExitStack
import numpy as np
import concourse.bass as bass
import concourse.mybir as mybir
from concourse.bass_utils import run_bass_kernel_spmd

F32 = mybir.dt.float32
BF16 = mybir.dt.bfloat16
AF = mybir.ActivationFunctionType
ALU = mybir.AluOpType
AX = mybir.AxisListType

D = 1024
KC = 8
DFF = 2816
JC = 22
NMEM = 256
MW = 512
M_COLS = 4 * MW + 8
R_W = 512
NCH = 31
DECAY_SCALE = math.exp(-0.5)
NORM_EPS = 1e-6
GN_EPS = 64e-5
TB = 512
SBK = 256
RC = 64


class Buf:
    __slots__ = ("w", "r")

    def __init__(self):
        self.w = None
        self.r = []


class V:
    def __init__(self, ap, bufs):
        self.ap = ap
        self.bufs = bufs

    def __getitem__(self, idx):
        return V(self.ap[idx], self.bufs)

    def re(self, pat, **kw):
        return V(self.ap.rearrange(pat, **kw), self.bufs)


class Tl:
    def __init__(self, t):
        self.t = t
        self.bufs = [Buf()]

    def __getitem__(self, idx):
        return V(self.t[idx], self.bufs)


class Ctx:
    ENG = ("pe", "act", "dve", "pool", "sp")

    def __init__(self, nc, stack):
        self.nc = nc
        self.stack = stack
        self.prog = {e: [] for e in self.ENG}
        self.sem = {}
        self.cnt = {}
        self.seen = {e: {} for e in self.ENG}
        for e in self.ENG:
            self.sem[e] = stack.enter_context(nc.semaphore("s_" + e))
            self.cnt[e] = 0
        self.dpool = {}
        for e in ("sp", "pool", "act"):
            keys = []
            for i in range(6):
                k = "d_%s%d" % (e, i)
                self.sem[k] = stack.enter_context(nc.semaphore(k))
                self.cnt[k] = 0
                keys.append(k)
            self.dpool[e] = [keys, 0]
        self.n_instr = 0
        self.sb_top = 16512
        self.sb_cap = 229344
        self.uid = 0
        self.psb = []
        self.psi = 0

    def sb(self, shape, dt, name=None):
        nbytes = int(np.prod(shape[1:])) * (4 if dt == F32 else 2)
        off = (self.sb_top + 63) // 64 * 64
        self.sb_top = off + nbytes
        assert self.sb_top <= self.sb_cap, ("SBUF overflow", self.sb_top)
        self.uid += 1
        t = self.nc.alloc_sbuf_tensor_at("%s_%d" % (name or "t", self.uid), list(shape), dt, offset=off)
        return Tl(t)

    def mark(self):
        return self.sb_top

    def release(self, m):
        self.sb_top = m

    def init_psum(self):
        for i in range(8):
            t = self.stack.enter_context(self.nc.psum_tensor("ps%d" % i, [128, 512], F32))
            self.psb.append(Tl(t))

    def psum(self):
        p = self.psb[self.psi % 8]
        self.psi += 1
        return p

    def emit(self, eng, fn, reads=(), writes=(), dma=False):
        reads = [r for r in reads if isinstance(r, V)]
        writes = [w for w in writes if isinstance(w, V)]
        need = {}

        def add(tk):
            if tk is None or (tk[0] == eng and eng == "pe"):
                return
            if need.get(tk[0], 0) < tk[1]:
                need[tk[0]] = tk[1]

        for v in reads:
            for b in v.bufs:
                add(b.w)
        for v in writes:
            for b in v.bufs:
                add(b.w)
                for tk in b.r:
                    add(tk)
        if dma:
            pl = self.dpool[eng]
            key = pl[0][pl[1] % len(pl[0])]
            pl[1] += 1
            inc = 16
            if self.cnt[key] > 0:
                need[key] = self.cnt[key]
        else:
            key, inc = eng, 1
        waits = []
        sn = self.seen[eng]
        for k, cval in need.items():
            if sn.get(k, 0) < cval:
                sn[k] = cval
                waits.append((self.sem[k], cval))
        self.cnt[key] += inc
        tk = (key, self.cnt[key])
        sem = self.sem[key]

        def run(e):
            for s, cval in waits:
                e.wait_ge(s, cval)
            fn(e).then_inc(sem, inc)

        self.prog[eng].append(run)
        self.n_instr += 1
        for v in reads:
            for b in v.bufs:
                b.r.append(tk)
        for v in writes:
            for b in v.bufs:
                b.w = tk
                b.r = []
        return tk

    def barrier(self, engs=None):
        for eng in (engs or self.ENG):
            items = []
            sn = self.seen[eng]
            for k, cval in self.cnt.items():
                if cval > 0 and k != eng and sn.get(k, 0) < cval:
                    sn[k] = cval
                    items.append((self.sem[k], cval))

            def run(e, items=items):
                for s, cval in items:
                    e.wait_ge(s, cval)

            self.prog[eng].append(run)

    def finish(self):
        nc = self.nc
        prog = self.prog
        with nc.Block() as block:
            @block.tensor
            def _(e):
                for f in prog["pe"]:
                    f(e)

            @block.scalar
            def _(e):
                for f in prog["act"]:
                    f(e)

            @block.vector
            def _(e):
                for f in prog["dve"]:
                    f(e)

            @block.gpsimd
            def _(e):
                for f in prog["pool"]:
                    f(e)

            @block.sync
            def _(e):
                for f in prog["sp"]:
                    f(e)

    def mm(self, out, lhsT, rhs, start=True, stop=True):
        o, l, r = out.ap, lhsT.ap, rhs.ap
        self.emit("pe", lambda e: e.matmul(o, lhsT=l, rhs=r, start=start, stop=stop),
                  reads=[lhsT, rhs] + ([] if start else [out]), writes=[out])

    def tr(self, out, in_, ident):
        o, i, d = out.ap, in_.ap, ident.ap
        self.emit("pe", lambda e: e.transpose(o, i, d), reads=[in_, ident], writes=[out])

    def act(self, out, in_, func, bias=0.0, scale=1.0, accum=None):
        o, i = out.ap, in_.ap
        b = bias.ap if isinstance(bias, V) else bias
        s = scale.ap if isinstance(scale, V) else scale
        if accum is None:
            self.emit("act", lambda e: e.activation(out=o, in_=i, func=func, bias=b, scale=s),
                      reads=[in_, bias, scale], writes=[out])
        else:
            a = accum.ap
            self.emit("act", lambda e: e.activation(out=o, in_=i, func=func, bias=b, scale=s, accum_out=a),
                      reads=[in_, bias, scale], writes=[out, accum])

    def tt(self, out, a, b, op, eng="dve"):
        o, x, y = out.ap, a.ap, b.ap
        self.emit(eng, lambda e: e.tensor_tensor(out=o, in0=x, in1=y, op=op), reads=[a, b], writes=[out])

    def ts(self, out, a, s1, s2=None, op0=ALU.mult, op1=None, eng="dve"):
        o, x = out.ap, a.ap
        p1 = s1.ap if isinstance(s1, V) else s1
        p2 = s2.ap if isinstance(s2, V) else s2
        if op1 is None:
            self.emit(eng, lambda e: e.tensor_scalar(out=o, in0=x, scalar1=p1, scalar2=None, op0=op0),
                      reads=[a, s1], writes=[out])
        else:
            self.emit(eng, lambda e: e.tensor_scalar(out=o, in0=x, scalar1=p1, scalar2=p2, op0=op0, op1=op1),
                      reads=[a, s1, s2], writes=[out])

    def stt(self, out, in0, scalar, in1, op0, op1):
        o, x, y = out.ap, in0.ap, in1.ap
        s = scalar.ap if isinstance(scalar, V) else scalar
        self.emit("dve", lambda e: e.scalar_tensor_tensor(out=o, in0=x, scalar=s, in1=y, op0=op0, op1=op1),
                  reads=[in0, scalar, in1], writes=[out])

    def cp(self, out, in_, eng="dve"):
        o, i = out.ap, in_.ap
        if eng == "act":
            self.emit("act", lambda e: e.activation(out=o, in_=i, func=AF.Copy), reads=[in_], writes=[out])
        else:
            self.emit(eng, lambda e: e.tensor_copy(out=o, in_=i), reads=[in_], writes=[out])

    def recip(self, out, in_):
        o, i = out.ap, in_.ap
        self.emit("dve", lambda e: e.reciprocal(out=o, in_=i), reads=[in_], writes=[out])

    def scan(self, out, d0, d1, init, op0, op1):
        o, x, y = out.ap, d0.ap, d1.ap
        ini = init.ap if isinstance(init, V) else init
        self.emit("dve", lambda e: e.tensor_tensor_scan(out=o, data0=x, data1=y, initial=ini, op0=op0, op1=op1),
                  reads=[d0, d1, init], writes=[out])

    def memset(self, out, val, eng="dve"):
        o = out.ap
        self.emit(eng, lambda e: e.memset(o, val), writes=[out])

    def dma(self, eng, out, in_, **kw):
        o = out.ap if isinstance(out, V) else out
        i = in_.ap if isinstance(in_, V) else in_
        return self.emit(eng, lambda e: e.dma_start(out=o, in_=i, **kw), reads=[in_], writes=[out], dma=True)


PV_NMIX, PV_NX, PV_NFFN, PV_NMEM, PV_NFIN = 0, 8, 16, 24, 32
PV_MU = 40
PV_CW = 71
PV_CB = 103
PV_W0, PV_A0, PV_KK, PV_KA, PV_RK, PV_GG, PV_GB = 111, 115, 119, 123, 127, 131, 135
PV_FW = 139
PV_FB = 205
PV_BI, PV_BF = 227, 228
PV_EPS, PV_GNEPS, PV_ONE = 229, 230, 231
NP = 232


def chunk_cols(ch):
    RB = M_COLS
    idx = np.full(128, -1, np.int64)
    if ch == 0:
        idx[0:64] = RB + 1536 + np.arange(64)
        idx[64:128] = RB + 1600 + np.arange(64)
    elif ch == 1:
        idx[:] = RB + 1664 + np.arange(128)
    elif ch == 2:
        idx[0:32] = RB + 1792 + np.arange(32)
        idx[32:36] = 2048 + np.arange(4)
        idx[64:68] = 2052 + np.arange(4)
    elif ch < 19:
        h, r = divmod(ch - 3, 4)
        idx[:] = r * 512 + h * 128 + np.arange(128)
    else:
        j, r = divmod(ch - 19, 3)
        idx[:] = RB + r * 512 + j * 128 + np.arange(128)
    return idx


def prep_shared(inp, L):
    f = np.float32
    w_in = inp["w_in"]
    w_in_p = np.zeros((L, D, NCH * 128), f)
    for ch in range(NCH):
        idx = chunk_cols(ch)
        ok = idx >= 0
        w_in_p[:, :, ch * 128 + np.nonzero(ok)[0]] = w_in[:L][:, :, idx[ok]]
    wa_up = np.zeros((L, 128, 1024), f)
    wa_up[:, 0:64, 0:512] = inp["r_w_up"][:L]
    wa_up[:, 64:128, 512:1024] = inp["r_a_up"][:L]
    g_up_p = np.zeros((L, 256, 512), f)
    g_up_p[:, 0:160, :] = inp["r_g_up"][:L]
    pv = np.zeros((L, 128, NP), f)

    def fm(v, n):
        return v.reshape(L, n, 128).transpose(0, 2, 1)

    pv[:, :, PV_NMIX:PV_NMIX + 8] = fm(inp["norm_mix"][:L], 8)
    pv[:, :, PV_NX:PV_NX + 8] = fm(inp["norm_x"][:L], 8)
    pv[:, :, PV_NFFN:PV_NFFN + 8] = fm(inp["norm_ffn"][:L], 8)
    pv[:, :, PV_NMEM:PV_NMEM + 8] = fm(inp["norm_mem"][:L], 8)
    pv[:, :, PV_NFIN:PV_NFIN + 8] = fm(np.broadcast_to(inp["norm_final"], (L, D)), 8)
    mu = inp["r_mu"][:L]
    for ch in list(range(0, 3)) + list(range(19, 31)):
        idx = chunk_cols(ch)
        for p in range(128):
            if idx[p] >= M_COLS:
                pv[:, p, PV_MU + ch] = mu[:, idx[p] - M_COLS]
    cw = inp["m_conv_w"][:L]
    cb = inp["m_conv_b"][:L]
    for h in range(4):
        for qk in range(2):
            chn = qk * 512 + h * 128 + np.arange(128)
            for tap in range(4):
                pv[:, :, PV_CW + (h * 2 + qk) * 4 + tap] = cw[:, tap, chn]
            pv[:, :, PV_CB + h * 2 + qk] = cb[:, chn]
    pv[:, :, PV_W0:PV_W0 + 4] = fm(inp["r_w0"][:L], 4)
    pv[:, :, PV_A0:PV_A0 + 4] = fm(inp["r_a0"][:L], 4)
    pv[:, :, PV_KK:PV_KK + 4] = fm(inp["r_kk"][:L], 4)
    pv[:, :, PV_KA:PV_KA + 4] = fm(inp["r_ka"][:L], 4)
    pv[:, :, PV_RK:PV_RK + 4] = fm(inp["r_rk"][:L].reshape(L, 512), 4)
    pv[:, :, PV_GG:PV_GG + 4] = fm(inp["r_gn_g"][:L], 4)
    pv[:, :, PV_GB:PV_GB + 4] = fm(inp["r_gn_b"][:L], 4)
    fw = inp["f_conv_w"][:L]
    for tap in range(3):
        pv[:, :, PV_FW + tap * JC:PV_FW + (tap + 1) * JC] = fm(fw[:, tap], JC)
    pv[:, :, PV_FB:PV_FB + JC] = fm(inp["f_conv_b"][:L], JC)
    pv[:, 0:4, PV_BI] = inp["m_gate_b"][:L, 0:4]
    pv[:, 0:4, PV_BF] = inp["m_gate_b"][:L, 4:8]
    pv[:, :, PV_EPS] = NORM_EPS
    pv[:, :, PV_GNEPS] = GN_EPS
    pv[:, :, PV_ONE] = 1.0
    mg = np.ascontiguousarray(np.broadcast_to(inp["m_norm_g"][:L, None, :], (L, 128, 512))).astype(f)
    p = np.arange(128)
    hh, ss = p // 64, p % 64
    same = hh[:, None] == hh[None, :]
    mS = (same & (ss[:, None] < ss[None, :])).astype(f)
    mI = (same & (ss[:, None] <= ss[None, :])).astype(f)
    BO = same.astype(f)
    ident = np.eye(128, dtype=f)
    ones = np.ones((128, 128), f)
    kap = np.float32(128 ** -0.5)
    mlm = (p[:, None] <= p[None, :]).astype(f) * kap
    cf = np.concatenate([ident, ones, BO / np.float32(64.0), mlm], 1)
    cb16 = np.concatenate([ident, ones, BO,
                           mS, mI, mS, mI,
                           -mS, mI, -mS, mI,
                           -mS.T, -mS.T, -mS.T, -mS.T], 1)
    rm = np.ones((128, 512), f)
    rm[:, ::RC] = 0.0
    cf = np.concatenate([cf, rm, np.ones((128, 512), f)], 1)
    return dict(w_in_p=w_in_p, wa_up=wa_up, g_up_p=g_up_p, pv=pv, mg=mg, cf=np.ascontiguousarray(cf),
                cb16=np.ascontiguousarray(cb16.astype(f)),
                w_out=np.ascontiguousarray(inp["w_out"][:L]), x_wq=np.ascontiguousarray(inp["x_wq"][:L]),
                x_wkv=np.ascontiguousarray(inp["x_wkv"][:L]), x_wo=np.ascontiguousarray(inp["x_wo"][:L]),
                f_up=np.ascontiguousarray(inp["f_up"][:L]), f_down=np.ascontiguousarray(inp["f_down"][:L]))


class G3:
    def __init__(self, tl, nk, ntok, blk=512):
        self.t = tl.t
        self.blk = blk
        self.b = [[Buf() for _ in range((ntok + blk - 1) // blk)] for _ in range(nk)]

    def bufs(self, k0, k1, t0, t1):
        return [self.b[k][tb] for k in range(k0, k1) for tb in range(t0 // self.blk, (t1 - 1) // self.blk + 1)]

    def v(self, k, t0, t1):
        return V(self.t[:, k, t0:t1], self.bufs(k, k + 1, t0, t1))

    def vk(self, k0, k1, t0, t1):
        return V(self.t[:, k0:k1, t0:t1], self.bufs(k0, k1, t0, t1))


def build(NSEQ=2, L=4, SEQ=2048, do_mix=True, do_xat=True, do_ffn=True, do_ml=True, do_rw=True, debug=False):
    nc = bass.Bass("TRN2", target_bir_lowering=False)
    dumped = {}
    NT = NSEQ * SEQ
    NHB = SEQ // TB

    def dr(name, shape):
        return nc.dram_tensor(name, list(shape), F32, kind="ExternalInput").ap()

    x_d = dr("x", [NT, D])
    mem_d = dr("mem", [NSEQ * NMEM, D])
    w_in_d = dr("w_in_p", [L, D, NCH * 128])
    wa_d = dr("wa_up", [L, 128, 1024])
    gu_d = dr("g_up_p", [L, 256, 512])
    pv_d = dr("pv", [L, 128, NP])
    mg_d = dr("mg", [L, 128, 512])
    cf_d = dr("cf", [128, 1536])
    cb_d = dr("cb16", [128, 1920])
    wout_d = dr("w_out", [L, D, D])
    wq_d = dr("x_wq", [L, D, D])
    wkv_d = dr("x_wkv", [L, D, 2 * D])
    wo_d = dr("x_wo", [L, D, D])
    fup_d = dr("f_up", [L, D, 2 * DFF])
    fdn_d = dr("f_down", [L, DFF, D])
    out_d = nc.dram_tensor("out", [NT, D], F32, kind="ExternalOutput").ap()

    with ExitStack() as st:
        c = Ctx(nc, st)
        c.init_psum()
        XT = G3(c.sb([128, 8, SEQ], F32, "XT"), 8, SEQ)
        HN = G3(c.sb([128, 8, TB], BF16, "HN"), 8, TB)
        YT = G3(c.sb([128, 8, TB], BF16, "YT"), 8, TB)
        WS = [c.sb([128, 4096], BF16, "WS") for _ in range(4)]
        wsi = [0]
        MEMT = c.sb([128, 8, NMEM], BF16, "MEMT")
        CF = c.sb([128, 1536], F32, "CF")
        CB = c.sb([128, 1920], BF16, "CB")
        PV = c.sb([128, NP], F32, "PV")
        PX = c.sb([128, 8], F32, "PX")
        MG = c.sb([128, 512], F32, "MG")
        WA = c.sb([128, 1024], BF16, "WA")
        GU = c.sb([128, 2, 512], BF16, "GU")
        CAR = c.sb([128, NCH * 3], F32, "CAR")
        FCAR = c.sb([128, JC * 2], F32, "FCAR")
        CST = c.sb([128, 4, 130], F32, "CST")
        CSB = c.sb([128, 4, 130], BF16, "CSB")
        GCAR = c.sb([4, 2], F32, "GCAR")
        GT = c.sb([128, 8, 16], F32, "GT")
        DECB = c.sb([128, 32], F32, "DECB")
        TSF = c.sb([128, 4, 128], F32, "TSF")
        TSB = c.sb([128, 4, 128], BF16, "TSB")
        KTM = c.sb([128, 8, NMEM], BF16, "KTM")
        VM = c.sb([128, 2, D], BF16, "VM")
        SQ = [c.sb([128, 512], BF16, "SQ") for _ in range(2)]
        RS = c.sb([128, 512], F32, "RS")
        base_mark = c.mark()

        identf = CF[:, 0:128]
        onesf = CF[:, 128:256]
        bof64 = CF[:, 256:384]
        mlmask = CF[:, 384:512]
        RM = CF[:, 512:1024]
        ONES = CF[:, 1024:1536]
        identb = CB[:, 0:128]
        onesb = CB[:, 128:256]
        BOb = CB[:, 256:384]
        MSI2 = CB[:, 384:896]
        NMSI2 = CB[:, 896:1408]
        NMSL4 = CB[:, 1408:1920]

        c.dma("sp", CF[:, :], cf_d)
        c.dma("pool", CB[:, :], cb_d)

        def dump(name, v, shape):
            if not debug or name in dumped:
                return
            t = nc.dram_tensor("dbg_" + name, list(shape), F32, kind="ExternalOutput").ap()
            dumped[name] = shape
            c.dma("pool", t, v)

        def pvc(col, n=1, p0=0, p1=128):
            return PV[p0:p1, col:col + n]

        def load_w(src3, nk, n):
            s = WS[wsi[0] % 4]
            wsi[0] += 1
            dst = s[:, 0:nk * n].re("p (k c) -> p k c", c=n)
            c.dma("pool", dst, src3)
            return dst

        def wcols(wd, l, c0, n):
            return wd[l][:, c0:c0 + n].rearrange("(k p) c -> p k c", p=128)

        def rmsnorm(gcol, tok0):
            for tb in range(TB // 512):
                a, b = tok0 + tb * 512, tok0 + (tb + 1) * 512
                pss = c.psum()
                for kc in range(8):
                    sq = SQ[kc % 2]
                    c.act(sq[:, :], XT.v(kc, a, b), AF.Square)
                    c.mm(pss[:, :], onesb, sq[:, :], start=(kc == 0), stop=(kc == 7))
                c.act(RS[:, :], pss[:, :], AF.Sqrt, scale=1.0 / D, bias=pvc(PV_EPS))
                c.recip(RS[:, :], RS[:, :])
                for kc in range(8):
                    c.stt(HN.v(kc, tb * 512, (tb + 1) * 512), XT.v(kc, a, b), pvc(gcol + kc), RS[:, :],
                          ALU.mult, ALU.mult)

        def proj_fm(w, q, src, tb, nk=8):
            ps = c.psum()
            for kc in range(nk):
                c.mm(ps[:, :], w[:, kc, q * 128:(q + 1) * 128], src.v(kc, tb * 512, (tb + 1) * 512),
                     start=(kc == 0), stop=(kc == nk - 1))
            return ps

        def resid_proj(wd, l, src, tok0):
            for cg in range(2):
                w = load_w(wcols(wd, l, cg * 512, 512), 8, 512)
                for q in range(4):
                    for tb in range(TB // 512):
                        ps = proj_fm(w, q, src, tb)
                        xv = XT.v(cg * 4 + q, tok0 + tb * 512, tok0 + (tb + 1) * 512)
                        c.tt(xv, xv, ps[:, :], ALU.add)

        def load_x(s):
            m = c.mark()
            xin = [c.sb([128, D], F32, "xin") for _ in range(2)]
            for tt_ in range(SEQ // 128):
                xi = xin[tt_ % 2]
                r0 = s * SEQ + tt_ * 128
                c.dma("sp", xi[:, :], x_d[r0:r0 + 128, :])
                for half in range(2):
                    pb = c.psum()
                    for q in range(4):
                        kc = half * 4 + q
                        c.tr(pb[:, q * 128:(q + 1) * 128], xi[:, kc * 128:(kc + 1) * 128], identf)
                    c.cp(XT.vk(half * 4, half * 4 + 4, tt_ * 128, (tt_ + 1) * 128),
                         pb[:, :].re("p (q t) -> p q t", q=4), eng=("act" if half else "dve"))
            mrs = c.sb([128, 2], F32, "mrs")
            msq = c.sb([128, D], F32, "msq")
            for mc in range(NMEM // 128):
                xi = xin[mc % 2]
                r0 = s * NMEM + mc * 128
                c.dma("sp", xi[:, :], mem_d[r0:r0 + 128, :])
                c.memset(mrs[:, :], 0.0)
                c.act(msq[:, :], xi[:, :], AF.Square, accum=mrs[:, 0:1])
                c.act(mrs[:, 1:2], mrs[:, 0:1], AF.Sqrt, scale=1.0 / D, bias=pvc(PV_EPS))
                c.recip(mrs[:, 1:2], mrs[:, 1:2])
                c.ts(msq[:, :], xi[:, :], mrs[:, 1:2], None, ALU.mult)
                for half in range(2):
                    pb = c.psum()
                    for q in range(4):
                        kc = half * 4 + q
                        c.tr(pb[:, q * 128:(q + 1) * 128], msq[:, kc * 128:(kc + 1) * 128], identf)
                    c.cp(MEMT[:, half * 4:half * 4 + 4, mc * 128:(mc + 1) * 128],
                         pb[:, :].re("p (q t) -> p q t", q=4), eng="act")
            c.barrier()
            c.release(m)

        def store_out(s):
            m = c.mark()
            NF = c.sb([128, 8, 512], F32, "NF")
            ot = [c.sb([128, D], F32, "ot") for _ in range(2)]
            for tb in range(SEQ // 512):
                a, b = tb * 512, (tb + 1) * 512
                pss = c.psum()
                for kc in range(8):
                    sq = SQ[kc % 2]
                    c.act(sq[:, :], XT.v(kc, a, b), AF.Square)
                    c.mm(pss[:, :], onesb, sq[:, :], start=(kc == 0), stop=(kc == 7))
                c.act(RS[:, :], pss[:, :], AF.Sqrt, scale=1.0 / D, bias=pvc(PV_EPS))
                c.recip(RS[:, :], RS[:, :])
                for kc in range(8):
                    c.stt(NF[:, kc, :], XT.v(kc, a, b), pvc(PV_NFIN + kc), RS[:, :], ALU.mult, ALU.mult)
                for t4 in range(4):
                    o = ot[t4 % 2]
                    for half in range(2):
                        pb = c.psum()
                        for q in range(4):
                            kc = half * 4 + q
                            c.tr(pb[:, q * 128:(q + 1) * 128], NF[:, kc, t4 * 128:(t4 + 1) * 128], identf)
                        c.cp(o[:, half * 512:(half + 1) * 512], pb[:, :], eng=("act" if half else "dve"))
                    r0 = s * SEQ + tb * 512 + t4 * 128
                    c.dma("sp", out_d[r0:r0 + 128, :], o[:, :])
            c.barrier()
            c.release(m)

        def ffn(l, tok0, first):
            m = c.mark()
            ACTT = G3(c.sb([128, JC, TB], BF16, "ACTT"), JC, TB)
            GR = [c.sb([128, 2 + 512], F32, "GR") for _ in range(2)]
            T1 = [c.sb([128, 512], F32, "T1") for _ in range(2)]
            if first:
                c.memset(FCAR[:, :], 0.0)
            rmsnorm(PV_NFFN, tok0)
            it = 0
            for g0 in range(0, JC, 4):
                ng = min(4, JC - g0)
                wg = load_w(wcols(fup_d, l, g0 * 128, ng * 128), 8, ng * 128)
                wv = load_w(wcols(fup_d, l, DFF + g0 * 128, ng * 128), 8, ng * 128)
                for q in range(ng):
                    jc = g0 + q
                    for tb in range(TB // 512):
                        gr, t1 = GR[it % 2], T1[it % 2]
                        it += 1
                        pg = proj_fm(wg, q, HN, tb)
                        pvv = proj_fm(wv, q, HN, tb)
                        c.cp(gr[:, 0:2], FCAR[:, jc * 2:jc * 2 + 2])
                        c.cp(gr[:, 2:514], pg[:, :], eng="act")
                        c.act(t1[:, :], gr[:, 2:514], AF.Identity, scale=pvc(PV_FW + 2 * JC + jc), bias=pvc(PV_FB + jc))
                        c.stt(t1[:, :], gr[:, 1:513], pvc(PV_FW + JC + jc), t1[:, :], ALU.mult, ALU.add)
                        c.stt(t1[:, :], gr[:, 0:512], pvc(PV_FW + jc), t1[:, :], ALU.mult, ALU.add)
                        c.cp(FCAR[:, jc * 2:jc * 2 + 2], gr[:, 512:514], eng="act")
                        c.act(t1[:, :], t1[:, :], AF.Silu)
                        c.tt(ACTT.v(jc, tb * 512, (tb + 1) * 512), t1[:, :], pvv[:, :], ALU.mult)
            for i in range(8):
                w = load_w(fdn_d[l][:, i * 128:(i + 1) * 128].rearrange("(j p) c -> p j c", p=128), JC, 128)
                for tb in range(TB // 512):
                    ps = c.psum()
                    for j in range(JC):
                        c.mm(ps[:, :], w[:, j, :], ACTT.v(j, tb * 512, (tb + 1) * 512), start=(j == 0), stop=(j == JC - 1))
                    xv = XT.v(i, tok0 + tb * 512, tok0 + (tb + 1) * 512)
                    c.tt(xv, xv, ps[:, :], ALU.add)
            c.barrier()
            c.release(m)

        def xattn_kv(l):
            m = c.mark()
            MN = G3(c.sb([128, 8, NMEM], BF16, "MN"), 8, NMEM)
            for kc in range(8):
                c.ts(MN.v(kc, 0, NMEM), MEMT[:, kc, :], pvc(PV_NMEM + kc), None, ALU.mult)
            for cg in range(2):
                w = load_w(wcols(wkv_d, l, cg * 512, 512), 8, 512)
                for q in range(4):
                    ps = c.psum()
                    for kc in range(8):
                        c.mm(ps[:, 0:NMEM], w[:, kc, q * 128:(q + 1) * 128], MN.v(kc, 0, NMEM), start=(kc == 0), stop=(kc == 7))
                    c.cp(KTM[:, cg * 4 + q, :], ps[:, 0:NMEM], eng="act")
            for cg in range(2):
                w = load_w(wcols(wkv_d, l, D + cg * 512, 512), 8, 512)
                for mc in range(2):
                    ps = c.psum()
                    for kc in range(8):
                        c.mm(ps[:, :], MN.v(kc, mc * 128, (mc + 1) * 128), w[:, kc, :], start=(kc == 0), stop=(kc == 7))
                    c.cp(VM[:, mc, cg * 512:(cg + 1) * 512], ps[:, :], eng="act")
            c.barrier()
            c.release(m)

        def xattn(l, tok0):
            m = c.mark()
            PT = [c.sb([128, 512], BF16, "PT") for _ in range(4)]
            RS2 = [c.sb([128, 512], F32, "RS2") for _ in range(2)]
            rmsnorm(PV_NX, tok0)
            for cg in range(2):
                w = load_w(wcols(wq_d, l, cg * 512, 512), 8, 512)
                for q in range(4):
                    for tb in range(TB // 512):
                        ps = proj_fm(w, q, HN, tb)
                        c.act(YT.v(cg * 4 + q, tb * 512, (tb + 1) * 512), ps[:, :], AF.Copy, scale=1.0 / 16.0)
            it = 0
            for h in range(4):
                for tb in range(TB // 512):
                    a, b = tb * 512, (tb + 1) * 512
                    pts = []
                    for mc in range(2):
                        ps = c.psum()
                        for dh in range(2):
                            c.mm(ps[:, :], KTM[:, 2 * h + dh, mc * 128:(mc + 1) * 128], YT.v(2 * h + dh, a, b),
                                 start=(dh == 0), stop=(dh == 1))
                        pt = PT[(it * 2 + mc) % 4]
                        c.act(pt[:, :], ps[:, :], AF.Exp)
                        pts.append(pt)
                    pss = c.psum()
                    for mc in range(2):
                        c.mm(pss[:, :], onesb, pts[mc][:, :], start=(mc == 0), stop=(mc == 1))
                    rs = RS2[it % 2]
                    c.recip(rs[:, :], pss[:, :])
                    for dh in range(2):
                        po = c.psum()
                        for mc in range(2):
                            c.mm(po[:, :], VM[:, mc, (2 * h + dh) * 128:(2 * h + dh + 1) * 128], pts[mc][:, :],
                                 start=(mc == 0), stop=(mc == 1))
                        c.tt(HN.v(2 * h + dh, a, b), po[:, :], rs[:, :], ALU.mult)
                    it += 1
            resid_proj(wo_d, l, HN, tok0)
            c.barrier()
            c.release(m)

        def mixer(l, tok0, first):
            m0 = c.mark()
            LA = c.sb([128, TB], BF16, "LA")
            SG1 = c.sb([128, TB], BF16, "SG1")
            SG2 = c.sb([128, TB], BF16, "SG2")
            RAW = [c.sb([128, 3 + 512], F32, "RAW") for _ in range(2)]
            TMP = [c.sb([128, 512], F32, "TMP") for _ in range(2)]
            rawi = [0]
            if first:
                c.memset(CAR[:, :], 0.0)
                c.memset(CST[:, :, :], 0.0)
                c.memset(CSB[:, :, :], 0.0, eng="pool")
                c.memset(GCAR[:, :], 0.0)
                c.memset(TSF[:, :, :], 0.0)
                c.memset(TSB[:, :, :], 0.0, eng="pool")
            rmsnorm(PV_NMIX, tok0)

            def proj_raw(w, q, ch, t0, n):
                ps = c.psum()
                for kc in range(8):
                    c.mm(ps[:, 0:n], w[:, kc, q * 128:(q + 1) * 128], HN.v(kc, t0, t0 + n), start=(kc == 0), stop=(kc == 7))
                raw = RAW[rawi[0] % 2]
                rawi[0] += 1
                c.cp(raw[:, 0:3], CAR[:, ch * 3:ch * 3 + 3])
                c.cp(raw[:, 3:3 + n], ps[:, 0:n], eng="act")
                c.cp(CAR[:, ch * 3:ch * 3 + 3], raw[:, n:n + 3], eng="act")
                return raw

            def proj_shift(w, q, ch, t0, n, dst):
                raw = proj_raw(w, q, ch, t0, n)
                tmp = TMP[rawi[0] % 2]
                c.tt(tmp[:, 0:n], raw[:, 2:2 + n], raw[:, 3:3 + n], ALU.subtract)
                c.stt(dst, tmp[:, 0:n], pvc(PV_MU + ch), raw[:, 3:3 + n], ALU.mult, ALU.add)

            m1 = c.mark()
            SH = [c.sb([128, 512], F32, "SH") for _ in range(2)]
            GI = c.sb([4, TB], F32, "GI")
            GF = c.sb([4, TB], F32, "GF")
            w = load_w(wcols(w_in_d, l, 0, 384), 8, 384)
            for sb in range(TB // 512):
                a, b = sb * 512, (sb + 1) * 512
                sh = SH[0]
                proj_shift(w, 0, 0, a, 512, sh[:, :])
                c.act(LA[0:64, a:b], sh[0:64, :], AF.Tanh)
                c.act(LA[64:128, a:b], sh[64:128, :], AF.Copy)
                sh = SH[1]
                proj_shift(w, 1, 1, a, 512, sh[:, :])
                c.act(SG1[:, a:b], sh[:, :], AF.Sigmoid)
                sh = SH[0]
                proj_shift(w, 2, 2, a, 512, sh[:, :])
                c.act(SG2[:, a:b], sh[:, :], AF.Sigmoid)
                c.dma("sp", GI[0:4, a:b], sh[32:36, :])
                c.dma("sp", GF[0:4, a:b], sh[64:68, :])

            if do_ml:
                mlstm(l, tok0, first, GI, GF, proj_raw, TMP)
            else:
                c.memset(YT.vk(0, 4, 0, TB), 0.0)
            c.barrier()
            c.release(m1)
            if do_rw:
                rwkv(l, tok0, first, LA, SG1, SG2, proj_shift)
            else:
                c.memset(YT.vk(4, 8, 0, TB), 0.0)
            c.barrier()
            c.release(m1)
            resid_proj(wout_d, l, YT, tok0)
            c.barrier()
            c.release(m0)

        def mlstm(l, tok0, first, GI, GF, proj_raw, TMP):
            NCK = TB // 128
            CS = c.sb([4, TB], F32, "CS")
            UM = c.sb([4, TB], F32, "UM")
            NMC = c.sb([4, 8], F32, "NMC")
            GS = c.sb([128, TB], F32, "GS")
            DG = c.sb([128, 8], F32, "DG")
            RH = c.sb([128, 32], F32, "RH")
            nbf = PX[0:4, 4:5]
            c.ts(nbf, pvc(PV_BF, 1, 0, 4), -1.0, None, ALU.mult)
            c.memset(GS[:, :], 0.0, eng="pool")
            c.memset(DG[:, :], 0.0, eng="pool")
            c.act(GF[:, :], GF[:, :], AF.Exp, scale=-1.0, bias=nbf)
            c.act(GF[:, :], GF[:, :], AF.Ln, scale=1.0, bias=pvc(PV_ONE, 1, 0, 4))
            for hb in range(TB // 512):
                a, b = hb * 512, (hb + 1) * 512
                ini = GCAR[:, 0:1] if hb == 0 else CS[:, a - 1:a]
                c.scan(CS[:, a:b], ONES[0:4, :], GF[:, a:b], ini, ALU.mult, ALU.add)
            c.stt(GI[:, :], GI[:, :], pvc(PV_BI, 1, 0, 4), CS[:, :], ALU.add, ALU.add)
            for hb in range(TB // 512):
                a, b = hb * 512, (hb + 1) * 512
                ini = GCAR[:, 1:2] if hb == 0 else UM[:, a - 1:a]
                c.scan(UM[:, a:b], GI[:, a:b], GI[:, a:b], ini, ALU.max, ALU.max)
            for ck in range(TB // 128):
                c.ts(NMC[:, ck:ck + 1], UM[:, ck * 128 + 127:ck * 128 + 128], -1.0, None, ALU.mult)
            c.tt(GF[:, :], CS[:, :], UM[:, :], ALU.subtract)
            c.act(GS[96:100, :], GF[:, :], AF.Exp)
            for ck in range(NCK):
                a, b = ck * 128, (ck + 1) * 128
                mc_ = UM[:, b - 1:b]
                nmc = NMC[:, ck:ck + 1]
                mp = GCAR[:, 1:2] if ck == 0 else UM[:, a - 1:a]
                c.act(GS[0:4, a:b], GI[:, a:b], AF.Exp, bias=nmc)
                c.act(GS[32:36, a:b], UM[:, a:b], AF.Exp, scale=-1.0, bias=mc_)
                c.act(GS[64:68, a:b], UM[:, a:b], AF.Exp, scale=-1.0, bias=mp)
                c.act(DG[0:4, ck:ck + 1], mp, AF.Exp, bias=nmc)
            for ck in range(NCK):
                pb = c.psum()
                c.tr(pb[:, 0:128], GS[:, ck * 128:(ck + 1) * 128], identf)
                c.cp(GT[:, ck, :].re("p (a b) -> p a b", b=4), pb[:, 0:128].re("p (a b) -> p a b", b=32)[:, :, 0:4])
            for h in range(4):
                c.ts(RH[:, h * 8:(h + 1) * 8], DG[:, :], identf[:, h:h + 1], None, ALU.mult)
            pb = c.psum()
            c.mm(pb[:, 0:32], onesf, RH[:, :])
            c.cp(DECB[:, :], pb[:, 0:32])
            dump("u", GI[:, :], [4, TB]); dump("cs", CS[:, :], [4, TB]); dump("um", UM[:, :], [4, TB])
            dump("gs", GS[:, :], [128, TB]); dump("gt", GT[:, :, :], [128, 8, 16]); dump("decb", DECB[:, :], [128, 32])
            c.cp(GCAR[:, 0:1], CS[:, TB - 1:TB])
            c.cp(GCAR[:, 1:2], UM[:, TB - 1:TB])
            QT = c.sb([128, TB], BF16, "QT")
            KT = c.sb([128, TB], BF16, "KT")
            VA = [c.sb([128, 130], BF16, "VA") for _ in range(2)]
            SO = [c.sb([128, 128], F32, "SO") for _ in range(2)]
            SM = [c.sb([128, 128], BF16, "SM") for _ in range(2)]
            KTK = [c.sb([128, 128], BF16, "KTK") for _ in range(2)]
            NU1 = [c.sb([128, 130], F32, "NU1") for _ in range(2)]
            NU2 = [c.sb([128, 130], F32, "NU2") for _ in range(2)]
            ST = [c.sb([128, 16], F32, "ST") for _ in range(2)]
            HH = [c.sb([128, 128], F32, "HH") for _ in range(2)]
            YM = [c.sb([128, 128], BF16, "YM") for _ in range(2)]
            it = 0
            for h in range(4):
                w = load_w(wcols(w_in_d, l, (3 + 4 * h) * 128, 512), 8, 512)
                for qk, dst in ((0, QT), (1, KT)):
                    ch = 3 + 4 * h + qk
                    cw = PV_CW + (h * 2 + qk) * 4
                    for sb in range(TB // 512):
                        raw = proj_raw(w, qk, ch, sb * 512, 512)
                        t = TMP[sb % 2]
                        c.act(t[:, :], raw[:, 3:515], AF.Identity, scale=pvc(cw + 3), bias=pvc(PV_CB + h * 2 + qk))
                        c.stt(t[:, :], raw[:, 2:514], pvc(cw + 2), t[:, :], ALU.mult, ALU.add)
                        c.stt(t[:, :], raw[:, 1:513], pvc(cw + 1), t[:, :], ALU.mult, ALU.add)
                        c.stt(t[:, :], raw[:, 0:512], pvc(cw + 0), t[:, :], ALU.mult, ALU.add)
                        c.act(dst[:, sb * 512:(sb + 1) * 512], t[:, :], AF.Silu)
                dump("qt", QT[:, :], [128, TB]); dump("kt", KT[:, :], [128, TB])
                for ck in range(NCK):
                    a, b = ck * 128, (ck + 1) * 128
                    i2 = it % 2
                    it += 1
                    va, so, sm, ktk, nu1, nu2, stt_, hh, ym = VA[i2], SO[i2], SM[i2], KTK[i2], NU1[i2], NU2[i2], ST[i2], HH[i2], YM[i2]
                    a_c = GT[:, ck, h:h + 1]
                    e_c = GT[:, ck, 4 + h:5 + h]
                    ep_c = GT[:, ck, 8 + h:9 + h]
                    fl_c = GT[:, ck, 12 + h:13 + h]
                    pvo = c.psum()
                    for kc in range(8):
                        c.mm(pvo[:, 0:256], HN.v(kc, a, b), w[:, kc, 256:512], start=(kc == 0), stop=(kc == 7))
                    c.ts(va[:, 0:128], pvo[:, 0:128], a_c, None, ALU.mult)
                    c.cp(va[:, 128:129], a_c)
                    c.act(so[:, :], pvo[:, 128:256], AF.Sigmoid)
                    pst = c.psum()
                    c.mm(pst[:, 0:128], KT[:, a:b], QT[:, a:b])
                    c.tt(sm[:, :], pst[:, 0:128], mlmask, ALU.mult)
                    p1 = c.psum()
                    c.mm(p1[:, 0:129], sm[:, :], va[:, 0:129])
                    c.mm(p1[:, 256:385], QT[:, a:b], CSB[:, h, 0:129])
                    c.ts(nu2[:, 0:129], p1[:, 256:385], ep_c, float(128 ** -0.5), ALU.mult, ALU.mult)
                    c.stt(nu1[:, 0:129], p1[:, 0:129], e_c, nu2[:, 0:129], ALU.mult, ALU.add)
                    c.stt(stt_[:, 0:1], nu1[:, 128:129], -1.0, nu1[:, 128:129], ALU.mult, ALU.max)
                    c.tt(stt_[:, 0:1], stt_[:, 0:1], fl_c, ALU.max)
                    c.recip(stt_[:, 1:2], stt_[:, 0:1])
                    c.ts(hh[:, :], nu1[:, 0:128], stt_[:, 1:2], None, ALU.mult)
                    c.emit("dve", (lambda o, i: (lambda e: e.bn_stats(out=o, in_=i)))(stt_.t[:, 2:8], hh.t[:, :]),
                           reads=[hh[:, :]], writes=[stt_[:, :]])
                    c.emit("dve", (lambda o, i: (lambda e: e.bn_aggr(out=o, in_=i)))(stt_.t[:, 8:10], stt_.t[:, 2:8]),
                           reads=[stt_[:, :]], writes=[stt_[:, :]])
                    c.act(stt_[:, 10:11], stt_[:, 9:10], AF.Sqrt, bias=pvc(PV_EPS))
                    c.recip(stt_[:, 10:11], stt_[:, 10:11])
                    c.ts(hh[:, :], hh[:, :], stt_[:, 8:9], stt_[:, 10:11], ALU.subtract, ALU.mult)
                    c.tt(hh[:, :], hh[:, :], so[:, :], ALU.mult)
                    c.tt(ym[:, :], hh[:, :], MG[:, h * 128:(h + 1) * 128], ALU.mult)
                    dump("va", va[:, 0:130], [128, 130]); dump("so", so[:, :], [128, 128]); dump("sm", sm[:, :], [128, 128])
                    dump("nu1", nu1[:, 0:130], [128, 130]); dump("ym", ym[:, :], [128, 128]); dump("st", stt_[:, :], [128, 16])
                    pT = c.psum()
                    c.mm(pT[:, 0:128], ym[:, :], identb)
                    c.cp(YT.v(h, a, b), pT[:, 0:128], eng="act")
                    c.mm(pT[:, 128:256], KT[:, a:b], identb)
                    c.cp(ktk[:, :], pT[:, 128:256], eng="act")
                    c.mm(pT[:, 256:385], ktk[:, :], va[:, 0:129])
                    c.stt(CST[:, h, 0:129], CST[:, h, 0:129], DECB[:, h * 8 + ck:h * 8 + ck + 1], pT[:, 256:385],
                          ALU.mult, ALU.add)
                    c.cp(CSB[:, h, 0:129], CST[:, h, 0:129], eng="act")

        def rwkv(l, tok0, first, LA, SG1, SG2, proj_shift):
            NCK = SBK // RC
            DS = DECAY_SCALE
            N = SBK

            def f32t():
                return c.sb([128, N], F32, "rf")

            RR, KR, VR, SW, AA, GG, CSW, EL, ENL, ELA, KK, T2, K2 = [f32t() for _ in range(13)]
            YR = CSW
            BV = SW
            KK2 = c.sb([128, N], BF16, "KK2")
            BS = c.sb([128, N], BF16, "BS")
            ARD = c.sb([128, NCK, 256], BF16, "ARD")
            KD = c.sb([128, NCK, 128], BF16, "KD")
            BD = c.sb([128, NCK, 128], BF16, "BD")
            VD = c.sb([128, NCK, 128], BF16, "VD")
            KDT = c.sb([128, NCK, 128], BF16, "KDT")
            BDT = c.sb([128, NCK, 128], BF16, "BDT")
            VDT = c.sb([128, NCK, 128], BF16, "VDT")
            G1 = c.sb([128, NCK, 256], BF16, "G1")
            G2 = c.sb([128, NCK, 256], BF16, "G2")
            XT_ = [c.sb([128, NCK, 128], BF16, "XTl") for _ in range(2)]
            XN_ = [c.sb([128, NCK, 128], BF16, "XNl") for _ in range(2)]
            PTt = c.sb([128, NCK, 128], BF16, "PTt")
            UE = [c.sb([128, 128], BF16, "UE") for _ in range(2)]
            NZ = [c.sb([128, 128], BF16, "NZ") for _ in range(2)]
            TT_ = c.sb([128, 128], F32, "TT_")
            for t in (ARD, KD, BD, VD):
                c.memset(t[:, :, :], 0.0, eng="pool")
            omka = PX[:, 0:4]
            c.ts(omka, pvc(PV_KA, 4), -1.0, 1.0, ALU.mult, ALU.add)

            def bd_write(dst, off, x, y):
                for hh_ in range(2):
                    p0, p1 = hh_ * 64, hh_ * 64 + 64
                    c.tt(dst[p0:p1, :, off + hh_ * 64:off + hh_ * 64 + 64],
                         x[p0:p1, :].re("p (c t) -> p c t", t=RC), y[p0:p1, :].re("p (c t) -> p c t", t=RC), ALU.mult)

            for j in range(4):
                w = load_w(wcols(w_in_d, l, (19 + 3 * j) * 128, 384), 8, 384)
                for sb in range(TB // SBK):
                    a, b = sb * SBK, (sb + 1) * SBK
                    proj_shift(w, 0, 19 + 3 * j, a, N, RR[:, :])
                    proj_shift(w, 1, 20 + 3 * j, a, N, KR[:, :])
                    proj_shift(w, 2, 21 + 3 * j, a, N, VR[:, :])
                    ps = c.psum()
                    c.mm(ps[:, 0:N], WA[:, j * 128:(j + 1) * 128], LA[:, a:b])
                    c.act(SW[:, :], ps[:, 0:N], AF.Sigmoid, bias=pvc(PV_W0 + j))
                    c.mm(ps[:, N:2 * N], WA[:, (4 + j) * 128:(5 + j) * 128], LA[:, a:b])
                    c.act(AA[:, :], ps[:, N:2 * N], AF.Sigmoid, bias=pvc(PV_A0 + j))
                    ps = c.psum()
                    c.mm(ps[:, 0:N], GU[:, 0, j * 128:(j + 1) * 128], SG1[:, a:b], start=True, stop=False)
                    c.mm(ps[:, 0:N], GU[:, 1, j * 128:(j + 1) * 128], SG2[:, a:b], start=False, stop=True)
                    c.cp(GG[:, :], ps[:, 0:N], eng="act")
                    c.scan(CSW[:, :], RM[:, 0:N], SW[:, :], 0.0, ALU.mult, ALU.add)
                    c.act(EL[:, :], CSW[:, :], AF.Exp, scale=-DS)
                    c.act(ENL[:, :], CSW[:, :], AF.Exp, scale=DS)
                    c.tt(T2[:, :], CSW[:, :], SW[:, :], ALU.subtract)
                    c.act(ELA[:, :], T2[:, :], AF.Exp, scale=-DS)
                    c.ts(KK[:, :], KR[:, :], pvc(PV_KK + j), None, ALU.mult)
                    c.act(KK2[:, :], KK[:, :], AF.Square)
                    ps = c.psum()
                    c.mm(ps[:, 0:N], BOb, KK2[:, :])
                    c.act(T2[:, :], ps[:, 0:N], AF.Sqrt)
                    c.ts(T2[:, :], T2[:, :], 1e-12, None, ALU.max)
                    c.recip(T2[:, :], T2[:, :])
                    c.tt(KK[:, :], KK[:, :], T2[:, :], ALU.mult)
                    c.ts(K2[:, :], AA[:, :], pvc(PV_KA + j), omka[:, j:j + 1], ALU.mult, ALU.add)
                    c.tt(K2[:, :], K2[:, :], KR[:, :], ALU.mult)
                    c.stt(BS[:, :], RR[:, :], pvc(PV_RK + j), K2[:, :], ALU.mult, ALU.mult)
                    c.mm(ps[:, N:2 * N], BOb, BS[:, :])
                    c.tt(BV[:, :], ps[:, N:2 * N], VR[:, :], ALU.mult)
                    bd_write(ARD, 128, RR, EL)
                    bd_write(KD, 0, K2, ENL)
                    bd_write(ARD, 0, KK, ELA)
                    c.tt(T2[:, :], KK[:, :], AA[:, :], ALU.mult)
                    bd_write(BD, 0, T2, ENL)
                    for hh_ in range(2):
                        p0, p1 = hh_ * 64, hh_ * 64 + 64
                        c.cp(VD[p0:p1, :, hh_ * 64:hh_ * 64 + 64], VR[p0:p1, :].re("p (c t) -> p c t", t=RC), eng="act")
                    for src, dst in ((KD, KDT), (BD, BDT), (VD, VDT)):
                        ps = c.psum()
                        for q in range(NCK):
                            c.mm(ps[:, q * 128:(q + 1) * 128], src[:, q, :], identb)
                        c.cp(dst[:, :, :], ps[:, :].re("p (q t) -> p q t", q=NCK), eng="act")
                    for qd in range(NCK // 2):
                        ps1 = c.psum()
                        ps2 = c.psum()
                        for u in range(2):
                            ci = qd * 2 + u
                            c.mm(ps1[:, u * 256:(u + 1) * 256], KD[:, ci, :], ARD[:, ci, :])
                            c.mm(ps2[:, u * 256:(u + 1) * 256], BD[:, ci, :], ARD[:, ci, :])
                        c.tt(G1[:, qd * 2:qd * 2 + 2, :], ps1[:, :].re("p (u t) -> p u t", u=2),
                             MSI2.re("p (u t) -> p u t", u=2), ALU.mult)
                        c.tt(G2[:, qd * 2:qd * 2 + 2, :], ps2[:, :].re("p (u t) -> p u t", u=2),
                             NMSI2.re("p (u t) -> p u t", u=2), ALU.mult)
                    xt, xn = XT_[0], XN_[0]
                    ps3 = c.psum()
                    for q in range(NCK):
                        c.mm(ps3[:, q * 128:(q + 1) * 128], ARD[:, q, 0:128], BD[:, q, :])
                    c.tt(xn[:, :, :], ps3[:, :].re("p (q t) -> p q t", q=NCK),
                         NMSL4.re("p (q t) -> p q t", q=NCK), ALU.mult)
                    c.cp(xt[:, :, :], G2[:, :, 0:128], eng="pool")
                    for ci in range(NCK):
                        c.tt(PTt[:, ci, :], xt[:, ci, :], identb, ALU.add, eng="pool")
                    NLEV = 5
                    for lev in range(NLEV):
                        xt2, xn2 = XT_[(lev + 1) % 2], XN_[(lev + 1) % 2]
                        last = lev == NLEV - 1
                        pa = c.psum()
                        pb = c.psum() if not last else None
                        for q in range(NCK):
                            c.mm(pa[:, q * 128:(q + 1) * 128], xt[:, q, :], xn[:, q, :])
                            if not last:
                                c.mm(pb[:, q * 128:(q + 1) * 128], xn[:, q, :], xt[:, q, :])
                        c.cp(xn2[:, :, :], pa[:, :].re("p (q t) -> p q t", q=NCK), eng="act")
                        if not last:
                            c.cp(xt2[:, :, :], pb[:, :].re("p (q t) -> p q t", q=NCK), eng="dve")
                        pc = c.psum()
                        for q in range(NCK):
                            c.mm(pc[:, q * 128:(q + 1) * 128], xn2[:, q, :], PTt[:, q, :])
                        c.tt(PTt[:, :, :], PTt[:, :, :], pc[:, :].re("p (q t) -> p q t", q=NCK), ALU.add)
                        xt, xn = xt2, xn2
                    for ci in range(NCK):
                        i2 = ci % 2
                        ue, nz = UE[i2], NZ[i2]
                        c0 = ci * RC
                        pu = c.psum()
                        c.mm(pu[:, 0:128], G1[:, ci, 0:128], VDT[:, ci, :], start=True, stop=False)
                        c.mm(pu[:, 0:128], ARD[:, ci, 0:128], TSB[:, j, :], start=False, stop=True)
                        c.cp(ue[:, :], pu[:, 0:128], eng="act")
                        c.mm(pu[:, 128:256], PTt[:, ci, :], ue[:, :])
                        c.act(nz[:, :], pu[:, 128:256], AF.Copy, scale=-1.0)
                        py = c.psum()
                        c.mm(py[:, 0:128], TSB[:, j, :], ARD[:, ci, 128:256], start=True, stop=False)
                        c.mm(py[:, 0:128], VDT[:, ci, :], G1[:, ci, 128:256], start=False, stop=False)
                        c.mm(py[:, 0:128], nz[:, :], G2[:, ci, 128:256], start=False, stop=True)
                        c.cp(YR[0:64, c0:c0 + RC], py[0:64, 0:64], eng="act")
                        c.cp(YR[64:128, c0:c0 + RC], py[64:128, 64:128], eng="act")
                        pt_ = c.psum()
                        c.mm(pt_[:, 0:128], KDT[:, ci, :], VDT[:, ci, :], start=True, stop=False)
                        c.mm(pt_[:, 0:128], BDT[:, ci, :], nz[:, :], start=False, stop=True)
                        c.tt(TT_[:, :], pt_[:, 0:128], TSF[:, j, :], ALU.add)
                        wc = EL[:, c0 + RC - 1:c0 + RC]
                        c.ts(TSB[:, j, :], TT_[:, :], wc, None, ALU.mult)
                        c.ts(TSF[:, j, :], TT_[:, :], wc, None, ALU.mult)
                    ps = c.psum()
                    c.mm(ps[:, 0:N], bof64, YR[:, :])
                    c.tt(YR[:, :], YR[:, :], ps[:, 0:N], ALU.subtract)
                    c.act(T2[:, :], YR[:, :], AF.Square)
                    c.mm(ps[:, N:2 * N], bof64, T2[:, :])
                    c.act(T2[:, :], ps[:, N:2 * N], AF.Sqrt, bias=pvc(PV_GNEPS))
                    c.recip(T2[:, :], T2[:, :])
                    c.tt(YR[:, :], YR[:, :], T2[:, :], ALU.mult)
                    c.ts(YR[:, :], YR[:, :], pvc(PV_GG + j), pvc(PV_GB + j), ALU.mult, ALU.add)
                    c.tt(YR[:, :], YR[:, :], BV[:, :], ALU.add)
                    c.tt(YT.v(4 + j, a, b), YR[:, :], GG[:, :], ALU.mult)

        for s in range(NSEQ):
            c.dma("sp", PV[:, :], pv_d[0])
            load_x(s)
            for l in range(L):
                c.dma("sp", PV[:, :], pv_d[l])
                c.dma("sp", MG[:, :], mg_d[l])
                c.dma("pool", WA[:, :], wa_d[l])
                c.dma("pool", GU[:, :, :], gu_d[l].rearrange("(k p) c -> p k c", p=128))
                if do_xat:
                    xattn_kv(l)
                for hb in range(NHB):
                    tok0 = hb * TB
                    if do_mix:
                        mixer(l, tok0, hb == 0)
                    if do_xat:
                        xattn(l, tok0)
                    if do_ffn:
                        ffn(l, tok0, hb == 0)
            store_out(s)
        c.barrier()
        c.finish()
        print("instructions:", c.n_instr, "sbuf top:", c.sb_top)
    return nc


_NC_CACHE = {}


def kernel(**inputs):
    inp = {k: np.asarray(v) for k, v in inputs.items()}
    L = 4
    shared = prep_shared(inp, L)
    if "nc" not in _NC_CACHE:
        _NC_CACHE["nc"] = build(NSEQ=2, L=L, SEQ=2048)
    nc = _NC_CACHE["nc"]
    x = np.ascontiguousarray(inp["x"], dtype=np.float32)
    mem = np.ascontiguousarray(inp["mem"], dtype=np.float32)
    in_maps = []
    for core in range(8):
        m = dict(shared)
        m["x"] = np.ascontiguousarray(x[2 * core:2 * core + 2].reshape(2 * 2048, D))
        m["mem"] = np.ascontiguousarray(mem[2 * core:2 * core + 2].reshape(2 * NMEM, D))
        in_maps.append(m)
    res = run_bass_kernel_spmd(nc, in_maps, core_ids=list(range(8)))
    out = np.concatenate([np.asarray(r["out"]).reshape(2, 2048, D) for r in res.results], 0)
    return out.astype(np.float32)
```

```python
import math
import threading
from contextlib import ExitStack
import numpy as np
import concourse.bass as bass
import concourse.mybir as mybir
from concourse.bass_utils import run_bass_kernel_spmd

F32 = mybir.dt.float32
BF16 = mybir.dt.bfloat16
AF = mybir.ActivationFunctionType
ALU = mybir.AluOpType
AX = mybir.AxisListType

D = 1024
KC = 8
DFF = 2816
JC = 22
NMEM = 256
MW = 512
M_COLS = 4 * MW + 8
R_W = 512
NCH = 31
DECAY_SCALE = math.exp(-0.5)
NORM_EPS = 1e-6
GN_EPS = 64e-5
TB = 512
SBK = 256
RC = 64


class Buf:
    __slots__ = ("w", "r")

    def __init__(self):
        self.w = None
        self.r = []


class V:
    def __init__(self, ap, bufs):
        self.ap = ap
        self.bufs = bufs

    def __getitem__(self, idx):
        return V(self.ap[idx], self.bufs)

    def re(self, pat, **kw):
        return V(self.ap.rearrange(pat, **kw), self.bufs)


class Tl:
    def __init__(self, t):
        self.t = t
        self.bufs = [Buf()]

    def __getitem__(self, idx):
        return V(self.t[idx], self.bufs)


class Ctx:
    ENG = ("pe", "act", "dve", "pool", "sp")

    def __init__(self, nc, stack):
        self.nc = nc
        self.stack = stack
        self.prog = {e: [] for e in self.ENG}
        self.sem = {}
        self.cnt = {}
        self.seen = {e: {} for e in self.ENG}
        for e in self.ENG:
            self.sem[e] = stack.enter_context(nc.semaphore("s_" + e))
            self.cnt[e] = 0
        self.dpool = {}
        for e in ("sp", "pool", "act"):
            keys = []
            for i in range(6):
                k = "d_%s%d" % (e, i)
                self.sem[k] = stack.enter_context(nc.semaphore(k))
                self.cnt[k] = 0
                keys.append(k)
            self.dpool[e] = [keys, 0]
        self.n_instr = 0
        self.sb_top = 16512
        self.allocs = []
        self.sb_cap = 229344
        self.uid = 0
        self.psb = []
        self.psi = 0
        self.weaving = False
        self.hold = False
        self.w_cur = 0
        self.w_psi = {}
        self.w_switch = None

    def sb(self, shape, dt, name=None):
        nbytes = int(np.prod(shape[1:])) * (4 if dt == F32 else 2)
        off = (self.sb_top + 63) // 64 * 64
        self.sb_top = off + nbytes
        assert self.sb_top <= self.sb_cap, ("SBUF overflow", self.sb_top)
        self.uid += 1
        t = self.nc.alloc_sbuf_tensor_at("%s_%d" % (name or "t", self.uid), list(shape), dt, offset=off)
        tl = Tl(t)
        end = off + nbytes
        inh = {}
        keep = []
        for (o0, o1, holder) in self.allocs:
            if o0 < end and off < o1:
                for b in holder():
                    for tk in ([b.w] if b.w else []) + b.r:
                        if inh.get(tk[0], 0) < tk[1]:
                            inh[tk[0]] = tk[1]
            else:
                keep.append((o0, o1, holder))
        self.allocs = keep
        tl.bufs[0].r = list(inh.items())
        tl.holder = [tl.bufs]
        self.allocs.append((off, end, (lambda h=tl.holder: h[0])))
        return tl

    def mark(self):
        return self.sb_top

    def release(self, m):
        self.sb_top = m

    def init_psum(self):
        for i in range(8):
            t = self.stack.enter_context(self.nc.psum_tensor("ps%d" % i, [128, 512], F32))
            self.psb.append(Tl(t))

    def psum(self):
        if self.weaving:
            k = self.w_cur
            n = 8 // self.w_n
            i = self.w_psi.get(k, 0)
            self.w_psi[k] = i + 1
            return self.psb[k * n + i % n]
        p = self.psb[self.psi % 8]
        self.psi += 1
        return p

    def weave(self, fns):
        n = len(fns)
        evs = [threading.Event() for _ in range(n)]
        alive = [True] * n
        done = threading.Event()
        errs = []
        self.w_n = n
        self.w_cur = 0
        self.w_psi = {}

        def nxt(i):
            for d in range(1, n + 1):
                k = (i + d) % n
                if alive[k]:
                    return k
            return None

        def worker(i):
            evs[i].wait()
            evs[i].clear()
            try:
                fns[i]()
            except BaseException as e:
                errs.append(e)
            alive[i] = False
            k = nxt(i)
            if k is None:
                done.set()
            else:
                self.w_cur = k
                evs[k].set()

        def switch():
            i = self.w_cur
            k = nxt(i)
            if k is None or k == i:
                return
            self.w_cur = k
            evs[k].set()
            evs[i].wait()
            evs[i].clear()

        self.w_switch = switch
        ths = [threading.Thread(target=worker, args=(i,)) for i in range(n)]
        for t in ths:
            t.start()
        self.weaving = True
        evs[0].set()
        done.wait()
        self.weaving = False
        for t in ths:
            t.join()
        if errs:
            raise errs[0]

    def emit(self, eng, fn, reads=(), writes=(), dma=False):
        reads = [r for r in reads if isinstance(r, V)]
        writes = [w for w in writes if isinstance(w, V)]
        need = {}

        def add(tk):
            if tk is None or (tk[0] == eng and eng == "pe"):
                return
            if need.get(tk[0], 0) < tk[1]:
                need[tk[0]] = tk[1]

        for v in reads:
            for b in v.bufs:
                add(b.w)
        for v in writes:
            for b in v.bufs:
                add(b.w)
                for tk in b.r:
                    add(tk)
        if dma:
            pl = self.dpool[eng]
            key = pl[0][pl[1] % len(pl[0])]
            pl[1] += 1
            inc = 16
            if self.cnt[key] > 0:
                need[key] = self.cnt[key]
        else:
            key, inc = eng, 1
        waits = []
        sn = self.seen[eng]
        for k, cval in need.items():
            if sn.get(k, 0) < cval:
                sn[k] = cval
                waits.append((self.sem[k], cval))
        self.cnt[key] += inc
        tk = (key, self.cnt[key])
        sem = self.sem[key]

        def run(e):
            for s, cval in waits:
                e.wait_ge(s, cval)
            fn(e).then_inc(sem, inc)

        self.prog[eng].append(run)
        self.n_instr += 1
        for v in reads:
            for b in v.bufs:
                b.r.append(tk)
        for v in writes:
            for b in v.bufs:
                b.w = tk
                b.r = []
        if self.weaving and not self.hold:
            self.w_switch()
        return tk

    def barrier(self, engs=None):
        for eng in (engs or self.ENG):
            items = []
            sn = self.seen[eng]
            for k, cval in self.cnt.items():
                if cval > 0 and k != eng and sn.get(k, 0) < cval:
                    sn[k] = cval
                    items.append((self.sem[k], cval))

            def run(e, items=items):
                for s, cval in items:
                    e.wait_ge(s, cval)

            self.prog[eng].append(run)

    def finish(self):
        nc = self.nc
        prog = self.prog
        with nc.Block() as block:
            @block.tensor
            def _(e):
                for f in prog["pe"]:
                    f(e)

            @block.scalar
            def _(e):
                for f in prog["act"]:
                    f(e)

            @block.vector
            def _(e):
                for f in prog["dve"]:
                    f(e)

            @block.gpsimd
            def _(e):
                for f in prog["pool"]:
                    f(e)

            @block.sync
            def _(e):
                for f in prog["sp"]:
                    f(e)

    def mm(self, out, lhsT, rhs, start=True, stop=True):
        o, l, r = out.ap, lhsT.ap, rhs.ap
        self.hold = not stop
        self.emit("pe", lambda e: e.matmul(o, lhsT=l, rhs=r, start=start, stop=stop),
                  reads=[lhsT, rhs] + ([] if start else [out]), writes=[out])
        self.hold = False

    def tr(self, out, in_, ident):
        o, i, d = out.ap, in_.ap, ident.ap
        self.emit("pe", lambda e: e.transpose(o, i, d), reads=[in_, ident], writes=[out])

    def act(self, out, in_, func, bias=0.0, scale=1.0, accum=None):
        o, i = out.ap, in_.ap
        b = bias.ap if isinstance(bias, V) else bias
        s = scale.ap if isinstance(scale, V) else scale
        if accum is None:
            self.emit("act", lambda e: e.activation(out=o, in_=i, func=func, bias=b, scale=s),
                      reads=[in_, bias, scale], writes=[out])
        else:
            a = accum.ap
            self.emit("act", lambda e: e.activation(out=o, in_=i, func=func, bias=b, scale=s, accum_out=a),
                      reads=[in_, bias, scale], writes=[out, accum])

    def tt(self, out, a, b, op, eng="dve"):
        o, x, y = out.ap, a.ap, b.ap
        self.emit(eng, lambda e: e.tensor_tensor(out=o, in0=x, in1=y, op=op), reads=[a, b], writes=[out])

    def ts(self, out, a, s1, s2=None, op0=ALU.mult, op1=None, eng="dve"):
        o, x = out.ap, a.ap
        p1 = s1.ap if isinstance(s1, V) else s1
        p2 = s2.ap if isinstance(s2, V) else s2
        if op1 is None:
            self.emit(eng, lambda e: e.tensor_scalar(out=o, in0=x, scalar1=p1, scalar2=None, op0=op0),
                      reads=[a, s1], writes=[out])
        else:
            self.emit(eng, lambda e: e.tensor_scalar(out=o, in0=x, scalar1=p1, scalar2=p2, op0=op0, op1=op1),
                      reads=[a, s1, s2], writes=[out])

    def stt(self, out, in0, scalar, in1, op0, op1):
        o, x, y = out.ap, in0.ap, in1.ap
        s = scalar.ap if isinstance(scalar, V) else scalar
        self.emit("dve", lambda e: e.scalar_tensor_tensor(out=o, in0=x, scalar=s, in1=y, op0=op0, op1=op1),
                  reads=[in0, scalar, in1], writes=[out])

    def cp(self, out, in_, eng="dve"):
        o, i = out.ap, in_.ap
        if eng == "act":
            self.emit("act", lambda e: e.activation(out=o, in_=i, func=AF.Copy), reads=[in_], writes=[out])
        else:
            self.emit(eng, lambda e: e.tensor_copy(out=o, in_=i), reads=[in_], writes=[out])

    def recip(self, out, in_):
        o, i = out.ap, in_.ap
        self.emit("dve", lambda e: e.reciprocal(out=o, in_=i), reads=[in_], writes=[out])

    def scan(self, out, d0, d1, init, op0, op1):
        o, x, y = out.ap, d0.ap, d1.ap
        ini = init.ap if isinstance(init, V) else init
        self.emit("dve", lambda e: e.tensor_tensor_scan(out=o, data0=x, data1=y, initial=ini, op0=op0, op1=op1),
                  reads=[d0, d1, init], writes=[out])

    def memset(self, out, val, eng="dve"):
        o = out.ap
        self.emit(eng, lambda e: e.memset(o, val), writes=[out])

    def dma(self, eng, out, in_, **kw):
        o = out.ap if isinstance(out, V) else out
        i = in_.ap if isinstance(in_, V) else in_
        return self.emit(eng, lambda e: e.dma_start(out=o, in_=i, **kw), reads=[in_], writes=[out], dma=True)


PV_NMIX, PV_NX, PV_NFFN, PV_NMEM, PV_NFIN = 0, 8, 16, 24, 32
PV_MU = 40
PV_CW = 71
PV_CB = 103
PV_W0, PV_A0, PV_KK, PV_KA, PV_RK, PV_GG, PV_GB = 111, 115, 119, 123, 127, 131, 135
PV_FW = 139
PV_FB = 205
PV_BI, PV_BF = 227, 228
PV_EPS, PV_GNEPS, PV_ONE = 229, 230, 231
NP = 232


def chunk_cols(ch):
    RB = M_COLS
    idx = np.full(128, -1, np.int64)
    if ch == 0:
        idx[0:64] = RB + 1536 + np.arange(64)
        idx[64:128] = RB + 1600 + np.arange(64)
    elif ch == 1:
        idx[:] = RB + 1664 + np.arange(128)
    elif ch == 2:
        idx[0:32] = RB + 1792 + np.arange(32)
        idx[32:36] = 2048 + np.arange(4)
        idx[64:68] = 2052 + np.arange(4)
    elif ch < 19:
        h, r = divmod(ch - 3, 4)
        idx[:] = r * 512 + h * 128 + np.arange(128)
    else:
        j, r = divmod(ch - 19, 3)
        idx[:] = RB + r * 512 + j * 128 + np.arange(128)
    return idx


def prep_shared(inp, L):
    f = np.float32
    w_in = inp["w_in"]
    w_in_p = np.zeros((L, D, NCH * 128), f)
    for ch in range(NCH):
        idx = chunk_cols(ch)
        ok = idx >= 0
        w_in_p[:, :, ch * 128 + np.nonzero(ok)[0]] = w_in[:L][:, :, idx[ok]]
    wa_up = np.zeros((L, 128, 1024), f)
    wa_up[:, 0:64, 0:512] = inp["r_w_up"][:L]
    wa_up[:, 64:128, 512:1024] = inp["r_a_up"][:L]
    g_up_p = np.zeros((L, 256, 512), f)
    g_up_p[:, 0:160, :] = inp["r_g_up"][:L]
    pv = np.zeros((L, 128, NP), f)

    def fm(v, n):
        return v.reshape(L, n, 128).transpose(0, 2, 1)

    pv[:, :, PV_NMIX:PV_NMIX + 8] = fm(inp["norm_mix"][:L], 8)
    pv[:, :, PV_NX:PV_NX + 8] = fm(inp["norm_x"][:L], 8)
    pv[:, :, PV_NFFN:PV_NFFN + 8] = fm(inp["norm_ffn"][:L], 8)
    pv[:, :, PV_NMEM:PV_NMEM + 8] = fm(inp["norm_mem"][:L], 8)
    pv[:, :, PV_NFIN:PV_NFIN + 8] = fm(np.broadcast_to(inp["norm_final"], (L, D)), 8)
    mu = inp["r_mu"][:L]
    for ch in list(range(0, 3)) + list(range(19, 31)):
        idx = chunk_cols(ch)
        for p in range(128):
            if idx[p] >= M_COLS:
                pv[:, p, PV_MU + ch] = mu[:, idx[p] - M_COLS]
    cw = inp["m_conv_w"][:L]
    cb = inp["m_conv_b"][:L]
    for h in range(4):
        for qk in range(2):
            chn = qk * 512 + h * 128 + np.arange(128)
            for tap in range(4):
                pv[:, :, PV_CW + (h * 2 + qk) * 4 + tap] = cw[:, tap, chn]
            pv[:, :, PV_CB + h * 2 + qk] = cb[:, chn]
    pv[:, :, PV_W0:PV_W0 + 4] = fm(inp["r_w0"][:L], 4)
    pv[:, :, PV_A0:PV_A0 + 4] = fm(inp["r_a0"][:L], 4)
    pv[:, :, PV_KK:PV_KK + 4] = fm(inp["r_kk"][:L], 4)
    pv[:, :, PV_KA:PV_KA + 4] = fm(inp["r_ka"][:L], 4)
    pv[:, :, PV_RK:PV_RK + 4] = fm(inp["r_rk"][:L].reshape(L, 512), 4)
    pv[:, :, PV_GG:PV_GG + 4] = fm(inp["r_gn_g"][:L], 4)
    pv[:, :, PV_GB:PV_GB + 4] = fm(inp["r_gn_b"][:L], 4)
    fw = inp["f_conv_w"][:L]
    for tap in range(3):
        pv[:, :, PV_FW + tap * JC:PV_FW + (tap + 1) * JC] = fm(fw[:, tap], JC)
    pv[:, :, PV_FB:PV_FB + JC] = fm(inp["f_conv_b"][:L], JC)
    pv[:, 0:4, PV_BI] = inp["m_gate_b"][:L, 0:4]
    pv[:, 0:4, PV_BF] = inp["m_gate_b"][:L, 4:8]
    pv[:, :, PV_EPS] = NORM_EPS
    pv[:, :, PV_GNEPS] = GN_EPS
    pv[:, :, PV_ONE] = 1.0
    mg = np.ascontiguousarray(np.broadcast_to(inp["m_norm_g"][:L, None, :], (L, 128, 512))).astype(f)
    p = np.arange(128)
    hh, ss = p // 64, p % 64
    same = hh[:, None] == hh[None, :]
    mS = (same & (ss[:, None] < ss[None, :])).astype(f)
    mI = (same & (ss[:, None] <= ss[None, :])).astype(f)
    BO = same.astype(f)
    ident = np.eye(128, dtype=f)
    ones = np.ones((128, 128), f)
    kap = np.float32(128 ** -0.5)
    mlm = (p[:, None] <= p[None, :]).astype(f) * kap
    cf = np.concatenate([ident, ones, BO / np.float32(64.0), mlm], 1)
    cb16 = np.concatenate([ident, ones, BO,
                           mS, mI, mS, mI,
                           -mS, mI, -mS, mI,
                           -mS.T, -mS.T, -mS.T, -mS.T], 1)
    rm = np.ones((128, 512), f)
    rm[:, ::RC] = 0.0
    cf = np.concatenate([cf, rm, np.ones((128, 512), f)], 1)
    return dict(w_in_p=w_in_p, wa_up=wa_up, g_up_p=g_up_p, pv=pv, mg=mg, cf=np.ascontiguousarray(cf),
                cb16=np.ascontiguousarray(cb16.astype(f)),
                w_out=np.ascontiguousarray(inp["w_out"][:L]), x_wq=np.ascontiguousarray(inp["x_wq"][:L]),
                x_wkv=np.ascontiguousarray(inp["x_wkv"][:L]), x_wo=np.ascontiguousarray(inp["x_wo"][:L]),
                f_up=np.ascontiguousarray(inp["f_up"][:L]), f_down=np.ascontiguousarray(inp["f_down"][:L]))


class G3:
    def __init__(self, tl, nk, ntok, blk=512):
        self.t = tl.t
        self.blk = blk
        self.b = [[Buf() for _ in range((ntok + blk - 1) // blk)] for _ in range(nk)]
        allb = [x for row in self.b for x in row]
        for x in allb:
            x.r = list(tl.bufs[0].r)
        tl.holder[0] = allb

    def bufs(self, k0, k1, t0, t1):
        return [self.b[k][tb] for k in range(k0, k1) for tb in range(t0 // self.blk, (t1 - 1) // self.blk + 1)]

    def v(self, k, t0, t1):
        return V(self.t[:, k, t0:t1], self.bufs(k, k + 1, t0, t1))

    def vk(self, k0, k1, t0, t1):
        return V(self.t[:, k0:k1, t0:t1], self.bufs(k0, k1, t0, t1))


def build(NSEQ=2, L=4, SEQ=2048, do_mix=True, do_xat=True, do_ffn=True, do_ml=True, do_rw=True, debug=False):
    nc = bass.Bass("TRN2", target_bir_lowering=False)
    dumped = {}
    NT = NSEQ * SEQ
    NHB = SEQ // TB

    def dr(name, shape):
        return nc.dram_tensor(name, list(shape), F32, kind="ExternalInput").ap()

    x_d = dr("x", [NT, D])
    mem_d = dr("mem", [NSEQ * NMEM, D])
    w_in_d = dr("w_in_p", [L, D, NCH * 128])
    wa_d = dr("wa_up", [L, 128, 1024])
    gu_d = dr("g_up_p", [L, 256, 512])
    pv_d = dr("pv", [L, 128, NP])
    mg_d = dr("mg", [L, 128, 512])
    cf_d = dr("cf", [128, 1536])
    cb_d = dr("cb16", [128, 1920])
    wout_d = dr("w_out", [L, D, D])
    wq_d = dr("x_wq", [L, D, D])
    wkv_d = dr("x_wkv", [L, D, 2 * D])
    wo_d = dr("x_wo", [L, D, D])
    fup_d = dr("f_up", [L, D, 2 * DFF])
    fdn_d = dr("f_down", [L, DFF, D])
    out_d = nc.dram_tensor("out", [NT, D], F32, kind="ExternalOutput").ap()

    with ExitStack() as st:
        c = Ctx(nc, st)
        c.init_psum()
        XT = G3(c.sb([128, 8, SEQ], F32, "XT"), 8, SEQ)
        HN = G3(c.sb([128, 8, TB], BF16, "HN"), 8, TB)
        YT = G3(c.sb([128, 8, TB], BF16, "YT"), 8, TB)
        WS = [c.sb([128, 4096], BF16, "WS") for _ in range(4)]
        wsi = [0]
        MEMT = c.sb([128, 8, NMEM], BF16, "MEMT")
        CF = c.sb([128, 1536], F32, "CF")
        CB = c.sb([128, 1920], BF16, "CB")
        PV = c.sb([128, NP], F32, "PV")
        PX = c.sb([128, 8], F32, "PX")
        MG = c.sb([128, 512], F32, "MG")
        WA = c.sb([128, 1024], BF16, "WA")
        GU = c.sb([128, 2, 512], BF16, "GU")
        CAR = [c.sb([128, 4], F32, "CAR") for _ in range(NCH)]
        FCAR = c.sb([128, JC * 2], F32, "FCAR")
        CST = c.sb([128, 4, 130], F32, "CST")
        CSB = c.sb([128, 4, 130], BF16, "CSB")
        GCAR = c.sb([4, 2], F32, "GCAR")
        GT = c.sb([128, 8, 16], F32, "GT")
        DECB = c.sb([128, 32], F32, "DECB")
        TSF = c.sb([128, 4, 128], F32, "TSF")
        TSB = c.sb([128, 4, 128], BF16, "TSB")
        KTM = c.sb([128, 8, NMEM], BF16, "KTM")
        VM = c.sb([128, 2, D], BF16, "VM")
        SQ = [c.sb([128, 512], BF16, "SQ") for _ in range(2)]
        RS = c.sb([128, 512], F32, "RS")
        base_mark = c.mark()

        identf = CF[:, 0:128]
        onesf = CF[:, 128:256]
        bof64 = CF[:, 256:384]
        mlmask = CF[:, 384:512]
        RM = CF[:, 512:1024]
        ONES = CF[:, 1024:1536]
        identb = CB[:, 0:128]
        onesb = CB[:, 128:256]
        BOb = CB[:, 256:384]
        MSI2 = CB[:, 384:896]
        NMSI2 = CB[:, 896:1408]
        NMSL4 = CB[:, 1408:1920]

        c.dma("sp", CF[:, :], cf_d)
        c.dma("pool", CB[:, :], cb_d)

        def dump(name, v, shape):
            if not debug or name in dumped:
                return
            t = nc.dram_tensor("dbg_" + name, list(shape), F32, kind="ExternalOutput").ap()
            dumped[name] = shape
            c.dma("pool", t, v)

        def pvc(col, n=1, p0=0, p1=128):
            return PV[p0:p1, col:col + n]

        wsk = {}

        def load_w(src3, nk, n):
            if c.weaving:
                k = c.w_cur
                per = 4 // c.w_n
                i = wsk.get(k, 0)
                wsk[k] = i + 1
                s = WS[k * per + i % per]
            else:
                s = WS[wsi[0] % 4]
                wsi[0] += 1
            dst = s[:, 0:nk * n].re("p (k c) -> p k c", c=n)
            c.dma("pool", dst, src3)
            return dst

        def prefetch_iter(loaders, depth):
            loaded = []
            n = len(loaders)
            nx = 0
            for i in range(n):
                while nx < n and nx <= i + depth:
                    loaded.append(loaders[nx]())
                    nx += 1
                yield loaded[i]

        def wcols(wd, l, c0, n):
            return wd[l][:, c0:c0 + n].rearrange("(k p) c -> p k c", p=128)

        def rmsnorm(gcol, tok0):
            for tb in range(TB // 512):
                a, b = tok0 + tb * 512, tok0 + (tb + 1) * 512
                pss = c.psum()
                for kc in range(8):
                    sq = SQ[kc % 2]
                    c.act(sq[:, :], XT.v(kc, a, b), AF.Square)
                    c.mm(pss[:, :], onesb, sq[:, :], start=(kc == 0), stop=(kc == 7))
                c.act(RS[:, :], pss[:, :], AF.Sqrt, scale=1.0 / D, bias=pvc(PV_EPS))
                c.recip(RS[:, :], RS[:, :])
                for kc in range(8):
                    c.stt(HN.v(kc, tb * 512, (tb + 1) * 512), XT.v(kc, a, b), pvc(gcol + kc), RS[:, :],
                          ALU.mult, ALU.mult)

        def proj_fm(w, q, src, tb, nk=8):
            ps = c.psum()
            for kc in range(nk):
                c.mm(ps[:, :], w[:, kc, q * 128:(q + 1) * 128], src.v(kc, tb * 512, (tb + 1) * 512),
                     start=(kc == 0), stop=(kc == nk - 1))
            return ps

        def resid_proj(wd, l, src, tok0):
            ws = prefetch_iter([(lambda cg=cg: load_w(wcols(wd, l, cg * 512, 512), 8, 512)) for cg in range(2)], 1)
            for cg in range(2):
                w = next(ws)
                for q in range(4):
                    for tb in range(TB // 512):
                        ps = proj_fm(w, q, src, tb)
                        xv = XT.v(cg * 4 + q, tok0 + tb * 512, tok0 + (tb + 1) * 512)
                        c.tt(xv, xv, ps[:, :], ALU.add)

        def load_x(s):
            m = c.mark()
            xin = [c.sb([128, D], F32, "xin") for _ in range(2)]
            for tt_ in range(SEQ // 128):
                xi = xin[tt_ % 2]
                r0 = s * SEQ + tt_ * 128
                c.dma("sp", xi[:, :], x_d[r0:r0 + 128, :])
                for half in range(2):
                    pb = c.psum()
                    for q in range(4):
                        kc = half * 4 + q
                        c.tr(pb[:, q * 128:(q + 1) * 128], xi[:, kc * 128:(kc + 1) * 128], identf)
                    c.cp(XT.vk(half * 4, half * 4 + 4, tt_ * 128, (tt_ + 1) * 128),
                         pb[:, :].re("p (q t) -> p q t", q=4), eng=("act" if half else "dve"))
            mrs = c.sb([128, 2], F32, "mrs")
            msq = c.sb([128, D], F32, "msq")
            for mc in range(NMEM // 128):
                xi = xin[mc % 2]
                r0 = s * NMEM + mc * 128
                c.dma("sp", xi[:, :], mem_d[r0:r0 + 128, :])
                c.memset(mrs[:, :], 0.0)
                c.act(msq[:, :], xi[:, :], AF.Square, accum=mrs[:, 0:1])
                c.act(mrs[:, 1:2], mrs[:, 0:1], AF.Sqrt, scale=1.0 / D, bias=pvc(PV_EPS))
                c.recip(mrs[:, 1:2], mrs[:, 1:2])
                c.ts(msq[:, :], xi[:, :], mrs[:, 1:2], None, ALU.mult)
                for half in range(2):
                    pb = c.psum()
                    for q in range(4):
                        kc = half * 4 + q
                        c.tr(pb[:, q * 128:(q + 1) * 128], msq[:, kc * 128:(kc + 1) * 128], identf)
                    c.cp(MEMT[:, half * 4:half * 4 + 4, mc * 128:(mc + 1) * 128],
                         pb[:, :].re("p (q t) -> p q t", q=4), eng="act")
            c.release(m)

        def store_out(s):
            m = c.mark()
            NF = c.sb([128, 8, 512], F32, "NF")
            ot = [c.sb([128, D], F32, "ot") for _ in range(2)]
            for tb in range(SEQ // 512):
                a, b = tb * 512, (tb + 1) * 512
                pss = c.psum()
                for kc in range(8):
                    sq = SQ[kc % 2]
                    c.act(sq[:, :], XT.v(kc, a, b), AF.Square)
                    c.mm(pss[:, :], onesb, sq[:, :], start=(kc == 0), stop=(kc == 7))
                c.act(RS[:, :], pss[:, :], AF.Sqrt, scale=1.0 / D, bias=pvc(PV_EPS))
                c.recip(RS[:, :], RS[:, :])
                for kc in range(8):
                    c.stt(NF[:, kc, :], XT.v(kc, a, b), pvc(PV_NFIN + kc), RS[:, :], ALU.mult, ALU.mult)
                for t4 in range(4):
                    o = ot[t4 % 2]
                    for half in range(2):
                        pb = c.psum()
                        for q in range(4):
                            kc = half * 4 + q
                            c.tr(pb[:, q * 128:(q + 1) * 128], NF[:, kc, t4 * 128:(t4 + 1) * 128], identf)
                        c.cp(o[:, half * 512:(half + 1) * 512], pb[:, :], eng=("act" if half else "dve"))
                    r0 = s * SEQ + tb * 512 + t4 * 128
                    c.dma("sp", out_d[r0:r0 + 128, :], o[:, :])
            c.release(m)

        def ffn(l, tok0, first):
            m = c.mark()
            ACTT = G3(c.sb([128, JC, TB], BF16, "ACTT"), JC, TB)
            GR = [c.sb([128, 2 + 512], F32, "GR") for _ in range(2)]
            T1 = [c.sb([128, 512], F32, "T1") for _ in range(2)]
            if first:
                c.memset(FCAR[:, :], 0.0)
            rmsnorm(PV_NFFN, tok0)
            it = 0
            lds = []
            for g0 in range(0, JC, 4):
                ng = min(4, JC - g0)
                lds.append(lambda g0=g0, ng=ng: load_w(wcols(fup_d, l, g0 * 128, ng * 128), 8, ng * 128))
                lds.append(lambda g0=g0, ng=ng: load_w(wcols(fup_d, l, DFF + g0 * 128, ng * 128), 8, ng * 128))
            ws = prefetch_iter(lds, 2)
            for g0 in range(0, JC, 4):
                ng = min(4, JC - g0)
                wg = next(ws)
                wv = next(ws)
                for q in range(ng):
                    jc = g0 + q
                    for tb in range(TB // 512):
                        gr, t1 = GR[it % 2], T1[it % 2]
                        it += 1
                        pg = proj_fm(wg, q, HN, tb)
                        pvv = proj_fm(wv, q, HN, tb)
                        c.cp(gr[:, 0:2], FCAR[:, jc * 2:jc * 2 + 2])
                        c.cp(gr[:, 2:514], pg[:, :], eng="act")
                        c.act(t1[:, :], gr[:, 2:514], AF.Identity, scale=pvc(PV_FW + 2 * JC + jc), bias=pvc(PV_FB + jc))
                        c.stt(t1[:, :], gr[:, 1:513], pvc(PV_FW + JC + jc), t1[:, :], ALU.mult, ALU.add)
                        c.stt(t1[:, :], gr[:, 0:512], pvc(PV_FW + jc), t1[:, :], ALU.mult, ALU.add)
                        c.cp(FCAR[:, jc * 2:jc * 2 + 2], gr[:, 512:514], eng="act")
                        c.act(t1[:, :], t1[:, :], AF.Silu)
                        c.tt(ACTT.v(jc, tb * 512, (tb + 1) * 512), t1[:, :], pvv[:, :], ALU.mult)
            ws = prefetch_iter([(lambda i=i: load_w(fdn_d[l][:, i * 128:(i + 1) * 128].rearrange("(j p) c -> p j c", p=128), JC, 128))
                                for i in range(8)], 2)
            for i in range(8):
                w = next(ws)
                for tb in range(TB // 512):
                    ps = c.psum()
                    for j in range(JC):
                        c.mm(ps[:, :], w[:, j, :], ACTT.v(j, tb * 512, (tb + 1) * 512), start=(j == 0), stop=(j == JC - 1))
                    xv = XT.v(i, tok0 + tb * 512, tok0 + (tb + 1) * 512)
                    c.tt(xv, xv, ps[:, :], ALU.add)
            c.release(m)

        def xattn_kv(l):
            m = c.mark()
            MN = G3(c.sb([128, 8, NMEM], BF16, "MN"), 8, NMEM)
            for kc in range(8):
                c.ts(MN.v(kc, 0, NMEM), MEMT[:, kc, :], pvc(PV_NMEM + kc), None, ALU.mult)
            for cg in range(2):
                w = load_w(wcols(wkv_d, l, cg * 512, 512), 8, 512)
                for q in range(4):
                    ps = c.psum()
                    for kc in range(8):
                        c.mm(ps[:, 0:NMEM], w[:, kc, q * 128:(q + 1) * 128], MN.v(kc, 0, NMEM), start=(kc == 0), stop=(kc == 7))
                    c.cp(KTM[:, cg * 4 + q, :], ps[:, 0:NMEM], eng="act")
            for cg in range(2):
                w = load_w(wcols(wkv_d, l, D + cg * 512, 512), 8, 512)
                for mc in range(2):
                    ps = c.psum()
                    for kc in range(8):
                        c.mm(ps[:, :], MN.v(kc, mc * 128, (mc + 1) * 128), w[:, kc, :], start=(kc == 0), stop=(kc == 7))
                    c.cp(VM[:, mc, cg * 512:(cg + 1) * 512], ps[:, :], eng="act")
            c.release(m)

        def xattn(l, tok0):
            m = c.mark()
            PT = [c.sb([128, 512], BF16, "PT") for _ in range(4)]
            RS2 = [c.sb([128, 512], F32, "RS2") for _ in range(2)]
            rmsnorm(PV_NX, tok0)
            ws = prefetch_iter([(lambda cg=cg: load_w(wcols(wq_d, l, cg * 512, 512), 8, 512)) for cg in range(2)], 1)
            for cg in range(2):
                w = next(ws)
                for q in range(4):
                    for tb in range(TB // 512):
                        ps = proj_fm(w, q, HN, tb)
                        c.act(YT.v(cg * 4 + q, tb * 512, (tb + 1) * 512), ps[:, :], AF.Copy, scale=1.0 / 16.0)
            it = 0
            for h in range(4):
                for tb in range(TB // 512):
                    a, b = tb * 512, (tb + 1) * 512
                    pts = []
                    for mc in range(2):
                        ps = c.psum()
                        for dh in range(2):
                            c.mm(ps[:, :], KTM[:, 2 * h + dh, mc * 128:(mc + 1) * 128], YT.v(2 * h + dh, a, b),
                                 start=(dh == 0), stop=(dh == 1))
                        pt = PT[(it * 2 + mc) % 4]
                        c.act(pt[:, :], ps[:, :], AF.Exp)
                        pts.append(pt)
                    pss = c.psum()
                    for mc in range(2):
                        c.mm(pss[:, :], onesb, pts[mc][:, :], start=(mc == 0), stop=(mc == 1))
                    rs = RS2[it % 2]
                    c.recip(rs[:, :], pss[:, :])
                    for dh in range(2):
                        po = c.psum()
                        for mc in range(2):
                            c.mm(po[:, :], VM[:, mc, (2 * h + dh) * 128:(2 * h + dh + 1) * 128], pts[mc][:, :],
                                 start=(mc == 0), stop=(mc == 1))
                        c.tt(HN.v(2 * h + dh, a, b), po[:, :], rs[:, :], ALU.mult)
                    it += 1
            resid_proj(wo_d, l, HN, tok0)
            c.release(m)

        def mixer(l, tok0, first):
            m0 = c.mark()
            LA = c.sb([128, TB], BF16, "LA")
            SG1 = c.sb([128, TB], BF16, "SG1")
            SG2 = c.sb([128, TB], BF16, "SG2")
            RAW = [c.sb([128, 3 + 512], F32, "RAW") for _ in range(2)]
            TMP = [c.sb([128, 512], F32, "TMP") for _ in range(2)]
            if first:
                for cr in CAR:
                    c.memset(cr[:, :], 0.0)
                c.memset(CST[:, :, :], 0.0)
                c.memset(CSB[:, :, :], 0.0, eng="pool")
                c.memset(GCAR[:, :], 0.0)
                c.memset(TSF[:, :, :], 0.0)
                c.memset(TSB[:, :, :], 0.0, eng="pool")
            rmsnorm(PV_NMIX, tok0)

            def make_proj(RAW, TMP):
                rawi = [0]

                def proj_raw(w, q, ch, t0, n):
                    ps = c.psum()
                    for kc in range(8):
                        c.mm(ps[:, 0:n], w[:, kc, q * 128:(q + 1) * 128], HN.v(kc, t0, t0 + n), start=(kc == 0), stop=(kc == 7))
                    raw = RAW[rawi[0] % 2]
                    rawi[0] += 1
                    c.cp(raw[:, 0:3], CAR[ch][:, 0:3])
                    c.cp(raw[:, 3:3 + n], ps[:, 0:n], eng="act")
                    c.cp(CAR[ch][:, 0:3], raw[:, n:n + 3], eng="act")
                    return raw

                def proj_shift(w, q, ch, t0, n, dst):
                    raw = proj_raw(w, q, ch, t0, n)
                    tmp = TMP[rawi[0] % 2]
                    c.tt(tmp[:, 0:n], raw[:, 2:2 + n], raw[:, 3:3 + n], ALU.subtract)
                    c.stt(dst, tmp[:, 0:n], pvc(PV_MU + ch), raw[:, 3:3 + n], ALU.mult, ALU.add)

                return proj_raw, proj_shift

            proj_raw, proj_shift = make_proj(RAW, TMP)

            m1 = c.mark()
            SH = [c.sb([128, 512], F32, "SH") for _ in range(2)]
            GI = c.sb([4, TB], F32, "GI")
            GF = c.sb([4, TB], F32, "GF")
            w = load_w(wcols(w_in_d, l, 0, 384), 8, 384)
            for sb in range(TB // 512):
                a, b = sb * 512, (sb + 1) * 512
                sh = SH[0]
                proj_shift(w, 0, 0, a, 512, sh[:, :])
                c.act(LA[0:64, a:b], sh[0:64, :], AF.Tanh)
                c.act(LA[64:128, a:b], sh[64:128, :], AF.Copy)
                sh = SH[1]
                proj_shift(w, 1, 1, a, 512, sh[:, :])
                c.act(SG1[:, a:b], sh[:, :], AF.Sigmoid)
                sh = SH[0]
                proj_shift(w, 2, 2, a, 512, sh[:, :])
                c.act(SG2[:, a:b], sh[:, :], AF.Sigmoid)
                c.dma("sp", GI[0:4, a:b], sh[32:36, :])
                c.dma("sp", GF[0:4, a:b], sh[64:68, :])

            if do_ml:
                mlstm_gates(l, tok0, first, GI, GF)
            c.release(m1)
            jobs = []
            if do_ml:
                RAWm0 = c.sb([128, 3 + 512], F32, "RAWm")
                RAWm = [RAWm0, RAWm0]
                prm, _ = make_proj(RAWm, None)
                jobs.append(lambda: mlstm_heads(l, tok0, first, prm, None))
            else:
                c.memset(YT.vk(0, 4, 0, TB), 0.0)
            if do_rw:
                jobs.append(lambda: rwkv(l, tok0, first, LA, SG1, SG2, proj_shift))
            else:
                c.memset(YT.vk(4, 8, 0, TB), 0.0)
            if len(jobs) == 2:
                c.weave(jobs)
            elif jobs:
                jobs[0]()
            c.release(m1)
            resid_proj(wout_d, l, YT, tok0)
            c.release(m0)

        def mlstm_gates(l, tok0, first, GI, GF):
            NCK = TB // 128
            CS = c.sb([4, TB], F32, "CS")
            UM = c.sb([4, TB], F32, "UM")
            NMC = c.sb([4, 8], F32, "NMC")
            GS = c.sb([128, TB], F32, "GS")
            DG = c.sb([128, 8], F32, "DG")
            RH = c.sb([128, 32], F32, "RH")
            nbf = PX[0:4, 4:5]
            c.ts(nbf, pvc(PV_BF, 1, 0, 4), -1.0, None, ALU.mult)
            c.memset(GS[:, :], 0.0, eng="pool")
            c.memset(DG[:, :], 0.0, eng="pool")
            c.act(GF[:, :], GF[:, :], AF.Exp, scale=-1.0, bias=nbf)
            c.act(GF[:, :], GF[:, :], AF.Ln, scale=1.0, bias=pvc(PV_ONE, 1, 0, 4))
            for hb in range(TB // 512):
                a, b = hb * 512, (hb + 1) * 512
                ini = GCAR[:, 0:1] if hb == 0 else CS[:, a - 1:a]
                c.scan(CS[:, a:b], ONES[0:4, :], GF[:, a:b], ini, ALU.mult, ALU.add)
            c.stt(GI[:, :], GI[:, :], pvc(PV_BI, 1, 0, 4), CS[:, :], ALU.add, ALU.add)
            for hb in range(TB // 512):
                a, b = hb * 512, (hb + 1) * 512
                ini = GCAR[:, 1:2] if hb == 0 else UM[:, a - 1:a]
                c.scan(UM[:, a:b], GI[:, a:b], GI[:, a:b], ini, ALU.max, ALU.max)
            for ck in range(TB // 128):
                c.ts(NMC[:, ck:ck + 1], UM[:, ck * 128 + 127:ck * 128 + 128], -1.0, None, ALU.mult)
            c.tt(GF[:, :], CS[:, :], UM[:, :], ALU.subtract)
            c.act(GS[96:100, :], GF[:, :], AF.Exp)
            for ck in range(NCK):
                a, b = ck * 128, (ck + 1) * 128
                mc_ = UM[:, b - 1:b]
                nmc = NMC[:, ck:ck + 1]
                mp = GCAR[:, 1:2] if ck == 0 else UM[:, a - 1:a]
                c.act(GS[0:4, a:b], GI[:, a:b], AF.Exp, bias=nmc)
                c.act(GS[32:36, a:b], UM[:, a:b], AF.Exp, scale=-1.0, bias=mc_)
                c.act(GS[64:68, a:b], UM[:, a:b], AF.Exp, scale=-1.0, bias=mp)
                c.act(DG[0:4, ck:ck + 1], mp, AF.Exp, bias=nmc)
            for ck in range(NCK):
                pb = c.psum()
                c.tr(pb[:, 0:128], GS[:, ck * 128:(ck + 1) * 128], identf)
                c.cp(GT[:, ck, :].re("p (a b) -> p a b", b=4), pb[:, 0:128].re("p (a b) -> p a b", b=32)[:, :, 0:4])
            for h in range(4):
                c.ts(RH[:, h * 8:(h + 1) * 8], DG[:, :], identf[:, h:h + 1], None, ALU.mult)
            pb = c.psum()
            c.mm(pb[:, 0:32], onesf, RH[:, :])
            c.cp(DECB[:, :], pb[:, 0:32])
            dump("u", GI[:, :], [4, TB]); dump("cs", CS[:, :], [4, TB]); dump("um", UM[:, :], [4, TB])
            dump("gs", GS[:, :], [128, TB]); dump("gt", GT[:, :, :], [128, 8, 16]); dump("decb", DECB[:, :], [128, 32])
            c.cp(GCAR[:, 0:1], CS[:, TB - 1:TB])
            c.cp(GCAR[:, 1:2], UM[:, TB - 1:TB])
        def mlstm_heads(l, tok0, first, proj_raw, TMP):
            NCK = TB // 128
            QT = c.sb([128, TB], BF16, "QT")
            KT = c.sb([128, TB], BF16, "KT")
            CT0 = c.sb([128, 512], F32, "CT")
            CT = [CT0, CT0]
            VA = [c.sb([128, 130], BF16, "VA") for _ in range(2)]
            SO = [c.sb([128, 128], F32, "SO") for _ in range(2)]
            SM = [c.sb([128, 128], BF16, "SM") for _ in range(2)]
            KTK = [c.sb([128, 128], BF16, "KTK") for _ in range(2)]
            NU1 = [c.sb([128, 130], F32, "NU1") for _ in range(2)]
            NU2 = [c.sb([128, 130], F32, "NU2") for _ in range(2)]
            ST = [c.sb([128, 16], F32, "ST") for _ in range(2)]
            HH = [c.sb([128, 128], F32, "HH") for _ in range(2)]
            YM = [c.sb([128, 128], BF16, "YM") for _ in range(2)]
            it = 0
            wn = load_w(wcols(w_in_d, l, 3 * 128, 512), 8, 512)
            for h in range(4):
                w = wn
                if h < 3:
                    wn = load_w(wcols(w_in_d, l, (3 + 4 * (h + 1)) * 128, 512), 8, 512)
                for qk, dst in ((0, QT), (1, KT)):
                    ch = 3 + 4 * h + qk
                    cw = PV_CW + (h * 2 + qk) * 4
                    for sb in range(TB // 512):
                        raw = proj_raw(w, qk, ch, sb * 512, 512)
                        t = CT[sb % 2]
                        c.act(t[:, :], raw[:, 3:515], AF.Identity, scale=pvc(cw + 3), bias=pvc(PV_CB + h * 2 + qk))
                        c.stt(t[:, :], raw[:, 2:514], pvc(cw + 2), t[:, :], ALU.mult, ALU.add)
                        c.stt(t[:, :], raw[:, 1:513], pvc(cw + 1), t[:, :], ALU.mult, ALU.add)
                        c.stt(t[:, :], raw[:, 0:512], pvc(cw + 0), t[:, :], ALU.mult, ALU.add)
                        c.act(dst[:, sb * 512:(sb + 1) * 512], t[:, :], AF.Silu)
                dump("qt", QT[:, :], [128, TB]); dump("kt", KT[:, :], [128, TB])
                for ck in range(NCK):
                    a, b = ck * 128, (ck + 1) * 128
                    i2 = it % 2
                    it += 1
                    va, so, sm, ktk, nu1, nu2, stt_, hh, ym = VA[i2], SO[i2], SM[i2], KTK[i2], NU1[i2], NU2[i2], ST[i2], HH[i2], YM[i2]
                    a_c = GT[:, ck, h:h + 1]
                    e_c = GT[:, ck, 4 + h:5 + h]
                    ep_c = GT[:, ck, 8 + h:9 + h]
                    fl_c = GT[:, ck, 12 + h:13 + h]
                    pvo = c.psum()
                    for kc in range(8):
                        c.mm(pvo[:, 0:256], HN.v(kc, a, b), w[:, kc, 256:512], start=(kc == 0), stop=(kc == 7))
                    c.ts(va[:, 0:128], pvo[:, 0:128], a_c, None, ALU.mult)
                    c.cp(va[:, 128:129], a_c)
                    c.act(so[:, :], pvo[:, 128:256], AF.Sigmoid)
                    pst = c.psum()
                    c.mm(pst[:, 0:128], KT[:, a:b], QT[:, a:b])
                    c.tt(sm[:, :], pst[:, 0:128], mlmask, ALU.mult)
                    p1 = c.psum()
                    c.mm(p1[:, 0:129], sm[:, :], va[:, 0:129])
                    c.mm(p1[:, 256:385], QT[:, a:b], CSB[:, h, 0:129])
                    c.ts(nu2[:, 0:129], p1[:, 256:385], ep_c, float(128 ** -0.5), ALU.mult, ALU.mult)
                    c.stt(nu1[:, 0:129], p1[:, 0:129], e_c, nu2[:, 0:129], ALU.mult, ALU.add)
                    c.stt(stt_[:, 0:1], nu1[:, 128:129], -1.0, nu1[:, 128:129], ALU.mult, ALU.max)
                    c.tt(stt_[:, 0:1], stt_[:, 0:1], fl_c, ALU.max)
                    c.recip(stt_[:, 1:2], stt_[:, 0:1])
                    c.ts(hh[:, :], nu1[:, 0:128], stt_[:, 1:2], None, ALU.mult)
                    c.emit("dve", (lambda o, i: (lambda e: e.bn_stats(out=o, in_=i)))(stt_.t[:, 2:8], hh.t[:, :]),
                           reads=[hh[:, :]], writes=[stt_[:, :]])
                    c.emit("dve", (lambda o, i: (lambda e: e.bn_aggr(out=o, in_=i)))(stt_.t[:, 8:10], stt_.t[:, 2:8]),
                           reads=[stt_[:, :]], writes=[stt_[:, :]])
                    c.act(stt_[:, 10:11], stt_[:, 9:10], AF.Sqrt, bias=pvc(PV_EPS))
                    c.recip(stt_[:, 10:11], stt_[:, 10:11])
                    c.ts(hh[:, :], hh[:, :], stt_[:, 8:9], stt_[:, 10:11], ALU.subtract, ALU.mult)
                    c.tt(hh[:, :], hh[:, :], so[:, :], ALU.mult)
                    c.tt(ym[:, :], hh[:, :], MG[:, h * 128:(h + 1) * 128], ALU.mult)
                    dump("va", va[:, 0:130], [128, 130]); dump("so", so[:, :], [128, 128]); dump("sm", sm[:, :], [128, 128])
                    dump("nu1", nu1[:, 0:130], [128, 130]); dump("ym", ym[:, :], [128, 128]); dump("st", stt_[:, :], [128, 16])
                    pT = c.psum()
                    c.mm(pT[:, 0:128], ym[:, :], identb)
                    c.cp(YT.v(h, a, b), pT[:, 0:128], eng="act")
                    c.mm(pT[:, 128:256], KT[:, a:b], identb)
                    c.cp(ktk[:, :], pT[:, 128:256], eng="act")
                    c.mm(pT[:, 256:385], ktk[:, :], va[:, 0:129])
                    c.stt(CST[:, h, 0:129], CST[:, h, 0:129], DECB[:, h * 8 + ck:h * 8 + ck + 1], pT[:, 256:385],
                          ALU.mult, ALU.add)
                    c.cp(CSB[:, h, 0:129], CST[:, h, 0:129], eng="act")

        def rwkv(l, tok0, first, LA, SG1, SG2, proj_shift):
            NCK = SBK // RC
            DS = DECAY_SCALE
            N = SBK

            def f32t():
                return c.sb([128, N], F32, "rf")

            RR, KR, VR, SW, AA, CSW, EL, ENL, KK, T2, K2 = [f32t() for _ in range(11)]
            GG = ENL
            YR = CSW
            BV = f32t()
            KK2 = c.sb([128, N], BF16, "KK2")
            BS = KK2
            ARD = c.sb([128, NCK, 256], BF16, "ARD")
            KD = c.sb([128, NCK, 128], BF16, "KD")
            BD = c.sb([128, NCK, 128], BF16, "BD")
            VD = c.sb([128, NCK, 128], BF16, "VD")
            KDT = c.sb([128, NCK, 128], BF16, "KDT")
            BDT = c.sb([128, NCK, 128], BF16, "BDT")
            VDT = c.sb([128, NCK, 128], BF16, "VDT")
            G1 = c.sb([128, NCK, 256], BF16, "G1")
            G2 = c.sb([128, NCK, 256], BF16, "G2")
            XT_ = [c.sb([128, NCK, 128], BF16, "XTl"), KD]
            XN_ = [c.sb([128, NCK, 128], BF16, "XNl"), BD]
            PTt = c.sb([128, NCK, 128], BF16, "PTt")
            UE = [c.sb([128, 128], BF16, "UE") for _ in range(2)]
            NZ = [c.sb([128, 128], BF16, "NZ") for _ in range(2)]
            TT_ = c.sb([128, 128], F32, "TT_")
            for t in (ARD, KD, BD, VD):
                c.memset(t[:, :, :], 0.0)
            omka = PX[:, 0:4]
            c.ts(omka, pvc(PV_KA, 4), -1.0, 1.0, ALU.mult, ALU.add)

            def bd_write(dst, off, x, y):
                for hh_ in range(2):
                    p0, p1 = hh_ * 64, hh_ * 64 + 64
                    c.tt(dst[p0:p1, :, off + hh_ * 64:off + hh_ * 64 + 64],
                         x[p0:p1, :].re("p (c t) -> p c t", t=RC), y[p0:p1, :].re("p (c t) -> p c t", t=RC), ALU.mult)

            wn = load_w(wcols(w_in_d, l, 19 * 128, 384), 8, 384)
            for j in range(4):
                w = wn
                if j < 3:
                    wn = load_w(wcols(w_in_d, l, (19 + 3 * (j + 1)) * 128, 384), 8, 384)
                for sb in range(TB // SBK):
                    a, b = sb * SBK, (sb + 1) * SBK
                    proj_shift(w, 0, 19 + 3 * j, a, N, RR[:, :])
                    proj_shift(w, 1, 20 + 3 * j, a, N, KR[:, :])
                    proj_shift(w, 2, 21 + 3 * j, a, N, VR[:, :])
                    ps = c.psum()
                    c.mm(ps[:, 0:N], WA[:, j * 128:(j + 1) * 128], LA[:, a:b])
                    c.act(SW[:, :], ps[:, 0:N], AF.Sigmoid, bias=pvc(PV_W0 + j))
                    c.mm(ps[:, N:2 * N], WA[:, (4 + j) * 128:(5 + j) * 128], LA[:, a:b])
                    c.act(AA[:, :], ps[:, N:2 * N], AF.Sigmoid, bias=pvc(PV_A0 + j))
                    c.scan(CSW[:, :], RM[:, 0:N], SW[:, :], 0.0, ALU.mult, ALU.add)
                    c.act(EL[:, :], CSW[:, :], AF.Exp, scale=-DS)
                    c.act(ENL[:, :], CSW[:, :], AF.Exp, scale=DS)
                    c.ts(KK[:, :], KR[:, :], pvc(PV_KK + j), None, ALU.mult)
                    c.act(KK2[:, :], KK[:, :], AF.Square)
                    ps = c.psum()
                    c.mm(ps[:, 0:N], BOb, KK2[:, :])
                    c.act(T2[:, :], ps[:, 0:N], AF.Sqrt)
                    c.ts(T2[:, :], T2[:, :], 1e-12, None, ALU.max)
                    c.recip(T2[:, :], T2[:, :])
                    c.tt(KK[:, :], KK[:, :], T2[:, :], ALU.mult)
                    c.ts(K2[:, :], AA[:, :], pvc(PV_KA + j), omka[:, j:j + 1], ALU.mult, ALU.add)
                    c.tt(K2[:, :], K2[:, :], KR[:, :], ALU.mult)
                    c.stt(BS[:, :], RR[:, :], pvc(PV_RK + j), K2[:, :], ALU.mult, ALU.mult)
                    c.mm(ps[:, N:2 * N], BOb, BS[:, :])
                    c.tt(BV[:, :], ps[:, N:2 * N], VR[:, :], ALU.mult)
                    bd_write(ARD, 128, RR, EL)
                    bd_write(KD, 0, K2, ENL)
                    c.tt(T2[:, :], CSW[:, :], SW[:, :], ALU.subtract)
                    c.act(T2[:, :], T2[:, :], AF.Exp, scale=-DS)
                    bd_write(ARD, 0, KK, T2)
                    c.tt(T2[:, :], KK[:, :], AA[:, :], ALU.mult)
                    bd_write(BD, 0, T2, ENL)
                    for hh_ in range(2):
                        p0, p1 = hh_ * 64, hh_ * 64 + 64
                        c.cp(VD[p0:p1, :, hh_ * 64:hh_ * 64 + 64], VR[p0:p1, :].re("p (c t) -> p c t", t=RC), eng="act")
                    for src, dst in ((KD, KDT), (BD, BDT), (VD, VDT)):
                        ps = c.psum()
                        for q in range(NCK):
                            c.mm(ps[:, q * 128:(q + 1) * 128], src[:, q, :], identb)
                        c.cp(dst[:, :, :], ps[:, :].re("p (q t) -> p q t", q=NCK), eng="act")
                    for qd in range(NCK // 2):
                        ps1 = c.psum()
                        ps2 = c.psum()
                        for u in range(2):
                            ci = qd * 2 + u
                            c.mm(ps1[:, u * 256:(u + 1) * 256], KD[:, ci, :], ARD[:, ci, :])
                            c.mm(ps2[:, u * 256:(u + 1) * 256], BD[:, ci, :], ARD[:, ci, :])
                        c.tt(G1[:, qd * 2:qd * 2 + 2, :], ps1[:, :].re("p (u t) -> p u t", u=2),
                             MSI2.re("p (u t) -> p u t", u=2), ALU.mult)
                        c.tt(G2[:, qd * 2:qd * 2 + 2, :], ps2[:, :].re("p (u t) -> p u t", u=2),
                             NMSI2.re("p (u t) -> p u t", u=2), ALU.mult)
                    xt, xn = XT_[0], XN_[0]
                    ps3 = c.psum()
                    for q in range(NCK):
                        c.mm(ps3[:, q * 128:(q + 1) * 128], ARD[:, q, 0:128], BD[:, q, :])
                    c.tt(xn[:, :, :], ps3[:, :].re("p (q t) -> p q t", q=NCK),
                         NMSL4.re("p (q t) -> p q t", q=NCK), ALU.mult)
                    c.cp(xt[:, :, :], G2[:, :, 0:128], eng="act")
                    for ci in range(NCK):
                        c.tt(PTt[:, ci, :], xt[:, ci, :], identb, ALU.add)
                    NLEV = 5
                    for lev in range(NLEV):
                        xt2, xn2 = XT_[(lev + 1) % 2], XN_[(lev + 1) % 2]
                        last = lev == NLEV - 1
                        pa = c.psum()
                        pb = c.psum() if not last else None
                        for q in range(NCK):
                            c.mm(pa[:, q * 128:(q + 1) * 128], xt[:, q, :], xn[:, q, :])
                            if not last:
                                c.mm(pb[:, q * 128:(q + 1) * 128], xn[:, q, :], xt[:, q, :])
                        c.cp(xn2[:, :, :], pa[:, :].re("p (q t) -> p q t", q=NCK), eng="act")
                        if not last:
                            c.cp(xt2[:, :, :], pb[:, :].re("p (q t) -> p q t", q=NCK), eng="dve")
                        pc = c.psum()
                        for q in range(NCK):
                            c.mm(pc[:, q * 128:(q + 1) * 128], xn2[:, q, :], PTt[:, q, :])
                        c.tt(PTt[:, :, :], PTt[:, :, :], pc[:, :].re("p (q t) -> p q t", q=NCK), ALU.add)
                        xt, xn = xt2, xn2
                    for ci in range(NCK):
                        i2 = ci % 2
                        ue, nz = UE[i2], NZ[i2]
                        c0 = ci * RC
                        pu = c.psum()
                        c.mm(pu[:, 0:128], G1[:, ci, 0:128], VDT[:, ci, :], start=True, stop=False)
                        c.mm(pu[:, 0:128], ARD[:, ci, 0:128], TSB[:, j, :], start=False, stop=True)
                        c.cp(ue[:, :], pu[:, 0:128], eng="act")
                        c.mm(pu[:, 128:256], PTt[:, ci, :], ue[:, :])
                        c.act(nz[:, :], pu[:, 128:256], AF.Copy, scale=-1.0)
                        py = c.psum()
                        c.mm(py[:, 0:128], TSB[:, j, :], ARD[:, ci, 128:256], start=True, stop=False)
                        c.mm(py[:, 0:128], VDT[:, ci, :], G1[:, ci, 128:256], start=False, stop=False)
                        c.mm(py[:, 0:128], nz[:, :], G2[:, ci, 128:256], start=False, stop=True)
                        c.cp(YR[0:64, c0:c0 + RC], py[0:64, 0:64], eng="act")
                        c.cp(YR[64:128, c0:c0 + RC], py[64:128, 64:128], eng="act")
                        pt_ = c.psum()
                        c.mm(pt_[:, 0:128], KDT[:, ci, :], VDT[:, ci, :], start=True, stop=False)
                        c.mm(pt_[:, 0:128], BDT[:, ci, :], nz[:, :], start=False, stop=True)
                        c.tt(TT_[:, :], pt_[:, 0:128], TSF[:, j, :], ALU.add)
                        wc = EL[:, c0 + RC - 1:c0 + RC]
                        c.ts(TSB[:, j, :], TT_[:, :], wc, None, ALU.mult)
                        c.ts(TSF[:, j, :], TT_[:, :], wc, None, ALU.mult)
                    ps = c.psum()
                    c.mm(ps[:, 0:N], GU[:, 0, j * 128:(j + 1) * 128], SG1[:, a:b], start=True, stop=False)
                    c.mm(ps[:, 0:N], GU[:, 1, j * 128:(j + 1) * 128], SG2[:, a:b], start=False, stop=True)
                    c.cp(GG[:, :], ps[:, 0:N], eng="act")
                    ps = c.psum()
                    c.mm(ps[:, 0:N], bof64, YR[:, :])
                    c.tt(YR[:, :], YR[:, :], ps[:, 0:N], ALU.subtract)
                    c.act(T2[:, :], YR[:, :], AF.Square)
                    c.mm(ps[:, N:2 * N], bof64, T2[:, :])
                    c.act(T2[:, :], ps[:, N:2 * N], AF.Sqrt, bias=pvc(PV_GNEPS))
                    c.recip(T2[:, :], T2[:, :])
                    c.tt(YR[:, :], YR[:, :], T2[:, :], ALU.mult)
                    c.ts(YR[:, :], YR[:, :], pvc(PV_GG + j), pvc(PV_GB + j), ALU.mult, ALU.add)
                    c.tt(YR[:, :], YR[:, :], BV[:, :], ALU.add)
                    c.tt(YT.v(4 + j, a, b), YR[:, :], GG[:, :], ALU.mult)

        for s in range(NSEQ):
            c.dma("sp", PV[:, :], pv_d[0])
            load_x(s)
            for l in range(L):
                c.dma("sp", PV[:, :], pv_d[l])
                c.dma("sp", MG[:, :], mg_d[l])
                c.dma("pool", WA[:, :], wa_d[l])
                c.dma("pool", GU[:, :, :], gu_d[l].rearrange("(k p) c -> p k c", p=128))
                if do_xat:
                    xattn_kv(l)
                for hb in range(NHB):
                    tok0 = hb * TB
                    if do_mix:
                        mixer(l, tok0, hb == 0)
                    if do_xat:
                        xattn(l, tok0)
                    if do_ffn:
                        ffn(l, tok0, hb == 0)
            store_out(s)
        c.barrier()
        c.finish()
        print("instructions:", c.n_instr, "sbuf top:", c.sb_top)
    return nc


_NC_CACHE = {}


def kernel(**inputs):
    inp = {k: np.asarray(v) for k, v in inputs.items()}
    L = 4
    shared = prep_shared(inp, L)
    if "nc" not in _NC_CACHE:
        _NC_CACHE["nc"] = build(NSEQ=2, L=L, SEQ=2048)
    nc = _NC_CACHE["nc"]
    x = np.ascontiguousarray(inp["x"], dtype=np.float32)
    mem = np.ascontiguousarray(inp["mem"], dtype=np.float32)
    in_maps = []
    for core in range(8):
        m = dict(shared)
        m["x"] = np.ascontiguousarray(x[2 * core:2 * core + 2].reshape(2 * 2048, D))
        m["mem"] = np.ascontiguousarray(mem[2 * core:2 * core + 2].reshape(2 * NMEM, D))
        in_maps.append(m)
    res = run_bass_kernel_spmd(nc, in_maps, core_ids=list(range(8)))
    out = np.concatenate([np.asarray(r["out"]).reshape(2, 2048, D) for r in res.results], 0)
    return out.astype(np.float32)
```
